# Optimizing a Trainium2 kernel written in Bass

```python
import math
import jax
import jax.numpy as jnp
from jax import lax
import numpy as np

D_MODEL = 1024
BATCH = 8
SEQ = 4096
DEPTH = 4

GRID_W = 64
CTX_LEN = 256
HEAD_DIM = 64
ROPE_THETA = 10000.0
BLK = 128
EPS = 1e-6
NEG = -1e30

A_HEADS = 8
A_KV = 2
A_GROUPS = A_HEADS // A_KV
WINDOW = 128
B_HEADS = 8
B_KV = 2
B_GROUPS = B_HEADS // B_KV
C_HEADS = 4
M_HEADS = 16
M_HEAD_DIM = 64
M_INNER = M_HEADS * M_HEAD_DIM
M_GROUPS = 2
M_HPG = M_HEADS // M_GROUPS
M_STATE = 128
M_CHUNK = 128
CONV_W = 3
M_XBC = M_INNER + 2 * M_GROUPS * M_STATE

N_BRANCH = 4
BR_WIDTHS = (A_HEADS * HEAD_DIM, B_HEADS * HEAD_DIM, 2 * C_HEADS * HEAD_DIM, M_INNER)
MIX_W = A_HEADS * HEAD_DIM + B_HEADS * HEAD_DIM + 2 * C_HEADS * HEAD_DIM + M_INNER

IN_PARTS = (
    ("a_q", A_HEADS * HEAD_DIM), ("a_k", A_KV * HEAD_DIM), ("a_v", A_KV * HEAD_DIM),
    ("b_q", B_HEADS * HEAD_DIM), ("b_k", B_KV * HEAD_DIM), ("b_v", B_KV * HEAD_DIM),
    ("c_q", 2 * C_HEADS * HEAD_DIM), ("c_k", 2 * C_HEADS * HEAD_DIM), ("c_v", 2 * C_HEADS * HEAD_DIM),
    ("d_z", M_INNER), ("d_xbc", M_XBC), ("d_dt", 2 * M_HEADS),
    ("g_a", D_MODEL), ("g_b", D_MODEL), ("g_c", D_MODEL), ("g_d", D_MODEL),
)
IN_W = (A_HEADS + 2 * A_KV + B_HEADS + 2 * B_KV + 6 * C_HEADS) * HEAD_DIM + 2 * M_INNER + 2 * M_GROUPS * M_STATE + 2 * M_HEADS + N_BRANCH * D_MODEL
GATE_NAMES = ("g_a", "g_b", "g_c", "g_d")
CTX_KV_PARTS = ("a_k", "a_v", "b_k", "b_v", "c_k", "c_v", "d_xbc", "d_dt")

P_HEADS = 8
N_KEYS = 128
N_EXP = N_KEYS * N_KEYS
P_DK = 256
P_TOPK = 16
P_CHUNK = 128

kernel_name = "hybrid_gated_mixers_peer_trunk"


def rmsnorm(x, g=None):
    xf = x.astype(jnp.float32)
    y = xf * lax.rsqrt(jnp.mean(xf * xf, axis=-1, keepdims=True) + EPS)
    if g is not None:
        y = y * g.astype(jnp.float32)
    return y.astype(x.dtype)


def modulate(h, shift, scale):
    return h * (1.0 + scale) + shift


def in_proj(h, w, names=None):
    out = {}
    off = 0
    for name, width in IN_PARTS:
        if names is None or name in names:
            out[name] = h @ w[:, off:off + width]
        off += width
    return out


def axial_rope_tables(rows):
    row = jnp.repeat(jnp.arange(rows, dtype=jnp.float32), GRID_W)
    col = jnp.tile(jnp.arange(GRID_W, dtype=jnp.float32), rows)
    nq = HEAD_DIM // 4
    inv = ROPE_THETA ** (-jnp.arange(nq, dtype=jnp.float32) / nq)
    ar = row[:, None] * inv
    ac = col[:, None] * inv
    ang = jnp.concatenate([ar, ar, ac, ac], axis=-1)
    return jnp.cos(ang), jnp.sin(ang)


def rope(x, cos, sin):
    x1, x2, x3, x4 = jnp.split(x, 4, axis=-1)
    rot = jnp.concatenate([-x2, x1, -x4, x3], axis=-1)
    return x * cos[:, None, :].astype(x.dtype) + rot * sin[:, None, :].astype(x.dtype)


def gqa_scores(q, k):
    return jnp.einsum("bqkgd,bskd->bkgqs", q, k).astype(jnp.float32) * (HEAD_DIM ** -0.5)


def gqa_values(p, v):
    return jnp.einsum("bkgqs,bskd->bqkgd", p.astype(v.dtype), v)


def softmax_with_sink(s, sink):
    m = jnp.maximum(jnp.max(s, axis=-1, keepdims=True), sink)
    e = jnp.exp(s - m)
    return e / (jnp.sum(e, axis=-1, keepdims=True) + jnp.exp(sink - m))


def sweep_query_blocks(block_fn, n_tok):
    out = lax.map(block_fn, jnp.arange(n_tok // BLK))
    nb, b, blk, w = out.shape
    return jnp.swapaxes(out, 0, 1).reshape(b, nb * blk, w)


def window_attention(pc, px, sink, cos, sin, need_ctx):
    b, s, _ = px["a_q"].shape
    lc = pc["a_k"].shape[1]
    q = rope(px["a_q"].reshape(b, s, A_HEADS, HEAD_DIM), cos, sin).reshape(b, s, A_KV, A_GROUPS, HEAD_DIM)
    k = rope(px["a_k"].reshape(b, s, A_KV, HEAD_DIM), cos, sin)
    v = px["a_v"].reshape(b, s, A_KV, HEAD_DIM)
    kc = pc["a_k"].reshape(b, lc, A_KV, HEAD_DIM)
    vc = pc["a_v"].reshape(b, lc, A_KV, HEAD_DIM)
    sink_b = sink.astype(jnp.float32).reshape(1, A_KV, A_GROUPS, 1, 1)
    pad = ((0, 0), (BLK, BLK), (0, 0), (0, 0))
    kp = jnp.pad(k, pad)
    vp = jnp.pad(v, pad)

    def block(n):
        start = n * BLK
        qb = lax.dynamic_slice_in_dim(q, start, BLK, axis=1)
        kb = lax.dynamic_slice_in_dim(kp, start, 3 * BLK, axis=1)
        vb = lax.dynamic_slice_in_dim(vp, start, 3 * BLK, axis=1)
        qpos = start + jnp.arange(BLK)
        kpos = start - BLK + jnp.arange(3 * BLK)
        valid = (jnp.abs(qpos[:, None] - kpos[None, :]) <= WINDOW) & (kpos[None, :] >= 0) & (kpos[None, :] < s)
        s_loc = jnp.where(valid, gqa_scores(qb, kb), NEG)
        p = softmax_with_sink(jnp.concatenate([gqa_scores(qb, kc), s_loc], axis=-1), sink_b)
        o = gqa_values(p[..., :lc], vc) + gqa_values(p[..., lc:], vb)
        return o.reshape(b, BLK, A_HEADS * HEAD_DIM)

    o_x = sweep_query_blocks(block, s)
    o_c = None
    if need_ctx:
        qc = pc["a_q"].reshape(b, lc, A_KV, A_GROUPS, HEAD_DIM)
        o_c = gqa_values(softmax_with_sink(gqa_scores(qc, kc), sink_b), vc).reshape(b, lc, A_HEADS * HEAD_DIM)
    return o_c, o_x


def qknorm_attention(pc, px, qn_g, kn_g, cos, sin, need_ctx):
    b, s, _ = px["b_q"].shape
    lc = pc["b_k"].shape[1]
    q = rope(rmsnorm(px["b_q"].reshape(b, s, B_HEADS, HEAD_DIM), qn_g), cos, sin).reshape(b, s, B_KV, B_GROUPS, HEAD_DIM)
    k = rope(rmsnorm(px["b_k"].reshape(b, s, B_KV, HEAD_DIM), kn_g), cos, sin)
    v = px["b_v"].reshape(b, s, B_KV, HEAD_DIM)
    kc = rmsnorm(pc["b_k"].reshape(b, lc, B_KV, HEAD_DIM), kn_g)
    vc = pc["b_v"].reshape(b, lc, B_KV, HEAD_DIM)
    k_all = jnp.concatenate([kc, k], axis=1)
    v_all = jnp.concatenate([vc, v], axis=1)

    def block(n):
        qb = lax.dynamic_slice_in_dim(q, n * BLK, BLK, axis=1)
        p = jax.nn.softmax(gqa_scores(qb, k_all), axis=-1)
        return gqa_values(p, v_all).reshape(b, BLK, B_HEADS * HEAD_DIM)

    o_x = sweep_query_blocks(block, s)
    o_c = None
    if need_ctx:
        qc = rmsnorm(pc["b_q"].reshape(b, lc, B_HEADS, HEAD_DIM), qn_g).reshape(b, lc, B_KV, B_GROUPS, HEAD_DIM)
        o_c = gqa_values(jax.nn.softmax(gqa_scores(qc, kc), axis=-1), vc).reshape(b, lc, B_HEADS * HEAD_DIM)
    return o_c, o_x


def diff_attention(pc, px, lq1, lk1, lq2, lk2, subln_g, lam_init, cos, sin, need_ctx):
    b, s, _ = px["c_q"].shape
    lc = pc["c_k"].shape[1]

    def qk_lat(t):
        return rope(t.reshape(b, s, 2 * C_HEADS, HEAD_DIM), cos, sin).reshape(b, s, C_HEADS, 2, HEAD_DIM)

    q = qk_lat(px["c_q"])
    k = qk_lat(px["c_k"])
    v = px["c_v"].reshape(b, s, C_HEADS, 2 * HEAD_DIM)
    kc = pc["c_k"].reshape(b, lc, C_HEADS, 2, HEAD_DIM)
    vc = pc["c_v"].reshape(b, lc, C_HEADS, 2 * HEAD_DIM)
    k_all = jnp.concatenate([kc, k], axis=1)
    v_all = jnp.concatenate([vc, v], axis=1)
    f32 = jnp.float32
    lam = (jnp.exp(jnp.sum(lq1.astype(f32) * lk1.astype(f32)))
           - jnp.exp(jnp.sum(lq2.astype(f32) * lk2.astype(f32))) + lam_init)

    def attend(qb, keys, vals):
        sc = jnp.einsum("bqhtd,bshtd->bhtqs", qb, keys).astype(f32) * (HEAD_DIM ** -0.5)
        p = jax.nn.softmax(sc, axis=-1)
        w = (p[:, :, 0] - lam * p[:, :, 1]).astype(vals.dtype)
        o = jnp.einsum("bhqs,bshe->bqhe", w, vals)
        o = rmsnorm(o, subln_g) * (1.0 - lam_init)
        return o.reshape(o.shape[0], o.shape[1], C_HEADS * 2 * HEAD_DIM)

    def block(n):
        return attend(lax.dynamic_slice_in_dim(q, n * BLK, BLK, axis=1), k_all, v_all)

    o_x = sweep_query_blocks(block, s)
    o_c = None
    if need_ctx:
        o_c = attend(pc["c_q"].reshape(b, lc, C_HEADS, 2, HEAD_DIM), kc, vc)
    return o_c, o_x


def dwconv_centred(u, w, bias):
    ch = u.shape[-1]
    pad = CONV_W // 2
    y = lax.conv_general_dilated(u, w[:, None, :].astype(u.dtype), (1,), ((pad, pad),),
                                 dimension_numbers=("NWC", "WIO", "NWC"), feature_group_count=ch)
    return y + bias.astype(u.dtype)


def segsum_exp(a):
    cs = jnp.cumsum(a, axis=-1)
    t = a.shape[-1]
    mask = jnp.tril(jnp.ones((t, t), dtype=bool))
    return jnp.exp(jnp.where(mask, cs[..., :, None] - cs[..., None, :], -jnp.inf))


def ssd_scan(xs, dt, A, Bm, Cm, h0, need_y):
    b, L, g, e, p = xs.shape
    n = Bm.shape[-1]
    nc = L // M_CHUNK
    X = (xs * dt[..., None]).reshape(b, nc, M_CHUNK, g, e, p)
    a = jnp.moveaxis((dt * A).reshape(b, nc, M_CHUNK, g, e), 2, -1)
    a_cs = jnp.cumsum(a, axis=-1)
    Bc = Bm.reshape(b, nc, M_CHUNK, g, n)
    Cc = Cm.reshape(b, nc, M_CHUNK, g, n)
    decay_to_end = jnp.exp(a_cs[..., -1:] - a_cs)
    states = jnp.einsum("bclgn,bcgel,bclgep->bcgepn", Bc, decay_to_end, X)
    chunk_a = jnp.pad(a_cs[..., -1], ((0, 0), (1, 0), (0, 0), (0, 0)))
    decay_chunk = segsum_exp(jnp.moveaxis(chunk_a, 1, -1))
    all_states = jnp.concatenate([h0[:, None], states], axis=1)
    new_states = jnp.einsum("bgezc,bcgepn->bzgepn", decay_chunk, all_states)
    final = new_states[:, -1]
    if not need_y:
        return None, final
    Lmat = segsum_exp(a)
    CB = jnp.einsum("bclgn,bcsgn->bcgls", Cc, Bc)
    y_diag = jnp.einsum("bcgels,bcsgep->bclgep", CB[:, :, :, None] * Lmat, X)
    y_off = jnp.einsum("bclgn,bcgepn,bcgel->bclgep", Cc, new_states[:, :-1], jnp.exp(a_cs))
    return (y_diag + y_off).reshape(b, L, g, e, p), final


def mamba2_bidir(pc, px, conv_w, conv_b, dt_bias, a_log, d_skip, norm_g, need_ctx):
    f32 = jnp.float32
    gn = M_GROUPS * M_STATE
    A = -jnp.exp(a_log.astype(f32)).reshape(2, M_GROUPS, M_HPG)
    dtb = dt_bias.astype(f32).reshape(2, M_GROUPS, M_HPG)
    dsk = d_skip.astype(f32).reshape(M_GROUPS, M_HPG, 1)

    def prep(p):
        bb, ll, _ = p["d_xbc"].shape
        xbc = jax.nn.silu(dwconv_centred(p["d_xbc"], conv_w, conv_b)).astype(f32)
        xs = xbc[..., :M_INNER].reshape(bb, ll, M_GROUPS, M_HPG, M_HEAD_DIM)
        Bm = xbc[..., M_INNER:M_INNER + gn].reshape(bb, ll, M_GROUPS, M_STATE)
        Cm = xbc[..., M_INNER + gn:].reshape(bb, ll, M_GROUPS, M_STATE)
        dt = jax.nn.softplus(p["d_dt"].astype(f32).reshape(bb, ll, 2, M_GROUPS, M_HPG) + dtb)
        return xs, Bm, Cm, dt

    def flip(t):
        return jnp.flip(t, axis=1)

    xc, Bc, Cc, dtc = prep(pc)
    xx, Bx, Cx, dtx = prep(px)
    h0 = jnp.zeros((xc.shape[0], M_GROUPS, M_HPG, M_HEAD_DIM, M_STATE), f32)
    yc_f, sc_f = ssd_scan(xc, dtc[:, :, 0], A[0], Bc, Cc, h0, need_ctx)
    yc_b, sc_b = ssd_scan(flip(xc), flip(dtc[:, :, 1]), A[1], flip(Bc), flip(Cc), h0, need_ctx)
    yx_f, _ = ssd_scan(xx, dtx[:, :, 0], A[0], Bx, Cx, sc_f, True)
    yx_b, _ = ssd_scan(flip(xx), flip(dtx[:, :, 1]), A[1], flip(Bx), flip(Cx), sc_b, True)

    def finish(y_f, y_b_rev, xs, z):
        bb, ll = xs.shape[:2]
        y = y_f + flip(y_b_rev) + xs * dsk
        y = y.reshape(bb, ll, M_INNER) * jax.nn.silu(z.astype(f32))
        y = rmsnorm(y.reshape(bb, ll, M_GROUPS, M_INNER // M_GROUPS)).reshape(bb, ll, M_INNER)
        return (y * norm_g.astype(f32)).astype(z.dtype)

    o_x = finish(yx_f, yx_b, xx, px["d_z"])
    o_c = finish(yc_f, yc_b, xc, pc["d_z"]) if need_ctx else None
    return o_c, o_x


def merge_branches(outs, p, w_br, w_out):
    acc = None
    off = 0
    for o, width, gname in zip(outs, BR_WIDTHS, GATE_NAMES):
        term = jax.nn.sigmoid(p[gname]) * (o @ w_br[off:off + width])
        acc = term if acc is None else acc + term
        off += width
    return acc @ w_out


def peer_ffn(h, wq, sub_keys, eu, ev):
    shape = h.shape
    tok = h.reshape(-1, P_CHUNK, shape[-1])

    def chunk(t):
        q = (t @ wq).reshape(P_CHUNK, P_HEADS, 2, P_DK // 2)
        s = jnp.einsum("thjd,hjkd->thjk", q, sub_keys).astype(jnp.float32)
        sv, si = lax.top_k(s, P_TOPK)
        cand = sv[:, :, 0, :, None] + sv[:, :, 1, None, :]
        cidx = si[:, :, 0, :, None] * N_KEYS + si[:, :, 1, None, :]
        best, pos = lax.top_k(cand.reshape(P_CHUNK, P_HEADS, P_TOPK * P_TOPK), P_TOPK)
        eidx = jnp.take_along_axis(cidx.reshape(P_CHUNK, P_HEADS, P_TOPK * P_TOPK), pos, axis=-1)
        gate = jax.nn.softmax(best, axis=-1)
        u = jnp.take(eu, eidx, axis=0)
        act = jax.nn.gelu(jnp.einsum("thkd,td->thk", u, t).astype(jnp.float32), approximate=False)
        v = jnp.take(ev, eidx, axis=0)
        return jnp.einsum("thk,thkd->td", (gate * act).astype(v.dtype), v)

    return lax.map(chunk, tok).reshape(shape)


def setup_inputs(seed: int = 0) -> dict:
    key = jax.random.key(seed)
    ks = iter(jax.random.split(key, 40))
    f32 = jnp.float32
    L, D = DEPTH, D_MODEL

    def nrm(shape, scale):
        return jax.random.normal(next(ks), shape, f32) * scale

    dt = jnp.exp(jax.random.uniform(next(ks), (L, 2, M_HEADS), f32) * (math.log(0.1) - math.log(0.001)) + math.log(0.001))
    return {
        "x": nrm((BATCH, SEQ, D), 1.0),
        "c": nrm((BATCH, D), 1.0),
        "ctx": nrm((BATCH, CTX_LEN, D), 1.0),
        "c_ctx": nrm((D,), 1.0),
        "w_ada": nrm((L, D, 6 * D), 0.5 * D ** -0.5),
        "b_ada": nrm((L, 6 * D), 0.02),
        "g_norm1": 1.0 + nrm((L, D), 0.02),
        "g_norm2": 1.0 + nrm((L, D), 0.02),
        "w_in": nrm((L, D, IN_W), D ** -0.5),
        "a_sink": nrm((L, A_HEADS), 0.5),
        "b_qnorm": 1.0 + nrm((L, HEAD_DIM), 0.02),
        "b_knorm": 1.0 + nrm((L, HEAD_DIM), 0.02),
        "c_lam_q1": nrm((L, HEAD_DIM), 0.1),
        "c_lam_k1": nrm((L, HEAD_DIM), 0.1),
        "c_lam_q2": nrm((L, HEAD_DIM), 0.1),
        "c_lam_k2": nrm((L, HEAD_DIM), 0.1),
        "c_subln": 1.0 + nrm((L, 2 * HEAD_DIM), 0.02),
        "m_conv_w": nrm((L, CONV_W, M_XBC), CONV_W ** -0.5),
        "m_conv_b": nrm((L, M_XBC), 0.02),
        "m_dt_bias": dt + jnp.log(-jnp.expm1(-dt)),
        "m_a_log": jnp.log(jax.random.uniform(next(ks), (L, 2, M_HEADS), f32, minval=1.0, maxval=16.0)),
        "m_d": 1.0 + nrm((L, M_HEADS), 0.1),
        "m_norm": 1.0 + nrm((L, M_INNER), 0.02),
        "w_br": nrm((L, MIX_W, D), (MIX_W // N_BRANCH) ** -0.5),
        "w_out": nrm((L, D, D), D ** -0.5),
        "p_wq": nrm((L, D, P_HEADS * P_DK), D ** -0.5),
        "p_subkeys": nrm((L, P_HEADS, 2, N_KEYS, P_DK // 2), (P_DK // 2) ** -0.5),
        "p_u": nrm((L, N_EXP, D), D ** -0.5),
        "p_v": nrm((L, N_EXP, D), P_HEADS ** -0.5),
        "g_final": 1.0 + nrm((D,), 0.02),
    }


def reference(x, c, ctx, c_ctx, w_ada, b_ada, g_norm1, g_norm2, w_in, a_sink, b_qnorm, b_knorm,
              c_lam_q1, c_lam_k1, c_lam_q2, c_lam_k2, c_subln, m_conv_w, m_conv_b, m_dt_bias,
              m_a_log, m_d, m_norm, w_br, w_out, p_wq, p_subkeys, p_u, p_v, g_final):
    ROWS = x.shape[1] // GRID_W
    cos, sin = axial_rope_tables(ROWS)
    for l in range(DEPTH):
        need_ctx = l < DEPTH - 1
        mx = [t[:, None, :] for t in jnp.split(jax.nn.silu(c) @ w_ada[l] + b_ada[l], 6, axis=-1)]
        mc = jnp.split(jax.nn.silu(c_ctx) @ w_ada[l] + b_ada[l], 6, axis=-1)
        h_x = modulate(rmsnorm(x, g_norm1[l]), mx[0], mx[1])
        h_c = modulate(rmsnorm(ctx, g_norm1[l]), mc[0], mc[1])
        px = in_proj(h_x, w_in[l])
        pc = in_proj(h_c, w_in[l], None if need_ctx else CTX_KV_PARTS)
        a_c, a_x = window_attention(pc, px, a_sink[l], cos, sin, need_ctx)
        b_c, b_x = qknorm_attention(pc, px, b_qnorm[l], b_knorm[l], cos, sin, need_ctx)
        lam_init = 0.8 - 0.6 * math.exp(-0.3 * l)
        d_c, d_x = diff_attention(pc, px, c_lam_q1[l], c_lam_k1[l], c_lam_q2[l], c_lam_k2[l],
                                  c_subln[l], lam_init, cos, sin, need_ctx)
        s_c, s_x = mamba2_bidir(pc, px, m_conv_w[l], m_conv_b[l], m_dt_bias[l], m_a_log[l],
                                m_d[l], m_norm[l], need_ctx)
        x = x + mx[2] * merge_branches([a_x, b_x, d_x, s_x], px, w_br[l], w_out[l])
        if need_ctx:
            ctx = ctx + mc[2] * merge_branches([a_c, b_c, d_c, s_c], pc, w_br[l], w_out[l])
        x = x + mx[5] * peer_ffn(modulate(rmsnorm(x, g_norm2[l]), mx[3], mx[4]),
                                 p_wq[l], p_subkeys[l], p_u[l], p_v[l])
        if need_ctx:
            ctx = ctx + mc[5] * peer_ffn(modulate(rmsnorm(ctx, g_norm2[l]), mc[3], mc[4]),
                                         p_wq[l], p_subkeys[l], p_u[l], p_v[l])
    return rmsnorm(x, g_final)
```

```python
import math
import numpy as np
from contextlib import ExitStack
import concourse.bass as bass
import concourse.mybir as mybir
from concourse.bass_utils import run_bass_kernel_spmd

F32 = mybir.dt.float32
BF16 = mybir.dt.bfloat16
I32 = mybir.dt.int32
U32 = mybir.dt.uint32
AF = mybir.ActivationFunctionType
ALU = mybir.AluOpType
AX = mybir.AxisListType

D = 1024
HD = 64
EPS = 1e-6
IN_PARTS = (("a_q", 512), ("a_k", 128), ("a_v", 128), ("b_q", 512), ("b_k", 128), ("b_v", 128),
            ("c_q", 512), ("c_k", 512), ("c_v", 512), ("d_z", 1024), ("d_xbc", 1536), ("d_dt", 32),
            ("g_a", 1024), ("g_b", 1024), ("g_c", 1024), ("g_d", 1024))
OFF = {}
_o = 0
for _n, _w in IN_PARTS:
    OFF[_n] = _o
    _o += _w
IN_W = _o
ROTP = np.array([d + 16 if (d // 16) % 2 == 0 else d - 16 for d in range(64)])
SIGN = np.array([-1.0 if (d // 16) % 2 == 0 else 1.0 for d in range(64)], np.float32)
NFMG = 21
TMW = 1824
VW = 776
QK_AQ, QK_AK, QK_BQ, QK_BK, QK_CQ, QK_CK = 0, 4, 6, 10, 12, 16
NPP = 120
NFP = 1504


def fm_colmap():
    cols = []

    def pair(base, h0, h1):
        p = np.concatenate([base + h0 * 64 + np.arange(64), base + h1 * 64 + np.arange(64)])
        r = np.concatenate([base + h0 * 64 + ROTP, base + h1 * 64 + ROTP])
        cols.append(p)
        cols.append(r)

    for i in range(4):
        pair(OFF["a_q"], 2 * i, 2 * i + 1)
    for j in range(2):
        pair(OFF["a_k"], j, j)
    for i in range(4):
        pair(OFF["b_q"], 2 * i, 2 * i + 1)
    for j in range(2):
        pair(OFF["b_k"], j, j)
    for i in range(4):
        pair(OFF["c_q"], 2 * i, 2 * i + 1)
    for i in range(4):
        pair(OFF["c_k"], 2 * i, 2 * i + 1)
    for c in range(12):
        cols.append(OFF["d_xbc"] + c * 128 + np.arange(128))
    for c in range(32):
        cols.append(OFF["g_a"] + c * 128 + np.arange(128))
    return np.concatenate(cols)


def tm_colmap():
    return np.concatenate([OFF["a_v"] + np.arange(128), OFF["b_v"] + np.arange(128),
                           OFF["c_v"] + np.arange(512), OFF["d_z"] + np.arange(1024),
                           OFF["d_dt"] + np.arange(32)])


class Buf:
    __slots__ = ("t", "lastw", "readers", "dsem", "dcnt", "name", "cols")

    def __init__(self, t, name=""):
        self.t = t
        self.lastw = None
        self.readers = []
        self.dsem = None
        self.dcnt = 0
        self.name = name

    def __getitem__(self, idx):
        return self.t[idx]


class MK:
    ENG = ("pe", "act", "dve", "pool", "sp")

    def __init__(self, nc):
        self.nc = nc
        self.es = ExitStack()
        self.e = {"pe": nc.tensor, "act": nc.scalar, "dve": nc.vector, "pool": nc.gpsimd, "sp": nc.sync}
        self.sem = {k: self.es.enter_context(nc.semaphore("c_" + k)) for k in self.ENG}
        self.cnt = {k: 0 for k in self.ENG}
        self.waited = {k: {} for k in self.ENG}
        self.dsems = []
        self.free_dsems = []
        self.dma_bufs = []
        self.nwaits = 0
        self.ninst = 0
        self.skip = []

    def sb(self, es, name, shape, dt):
        self.nsb = getattr(self, "nsb", 0) + 1
        name = "s%d_%s" % (self.nsb, name)
        b = Buf(es.enter_context(self.nc.sbuf_tensor(name, list(shape), dt)), name)
        es.callback(self.release, [b])
        return b

    def _dsem(self, b):
        if b.dsem is None:
            if self.free_dsems:
                b.dsem, b.dcnt = self.free_dsems.pop()
            else:
                b.dsem = self.es.enter_context(self.nc.semaphore("d%d" % len(self.dsems)))
                self.dsems.append(b.dsem)
                b.dcnt = 0
            self.dma_bufs.append(b)
        return b.dsem

    def release(self, bufs):
        for b in bufs:
            if b.dsem is not None:
                self.free_dsems.append((b.dsem, b.dcnt))
                b.dsem = None
                self.dma_bufs.remove(b)

    def _wait(self, eng, tok):
        sem, val = tok
        if eng == "pe" and sem is self.sem["pe"]:
            return
        w = self.waited[eng]
        key = id(sem)
        if w.get(key, 0) >= val:
            return
        w[key] = val
        self.e[eng].wait_ge(sem, val)
        self.nwaits += 1

    def _deps(self, eng, reads, writes):
        for b in reads:
            if b.lastw is not None:
                self._wait(eng, b.lastw)
        for b in writes:
            if b.lastw is not None:
                self._wait(eng, b.lastw)
            for t in b.readers:
                self._wait(eng, t)

    def _commit(self, tok, reads, writes):
        for b in reads:
            b.readers.append(tok)
            if len(b.readers) > 48:
                best = {}
                for s, v in b.readers:
                    if best.get(id(s), (None, -1))[1] < v:
                        best[id(s)] = (s, v)
                b.readers = list(best.values())
        for b in writes:
            b.lastw = tok
            b.readers = []

    def op(self, eng, fn, reads=(), writes=()):
        self._deps(eng, reads, writes)
        ins = fn(self.e[eng])
        self.cnt[eng] += 1
        ins.then_inc(self.sem[eng], 1)
        tok = (self.sem[eng], self.cnt[eng])
        self._commit(tok, reads, writes)
        self.ninst += 1
        return tok

    def dma(self, eng, fn, reads=(), writes=()):
        self._deps(eng, reads, writes)
        bufs = list(writes) + list(reads)
        b0 = bufs[0]
        sem = self._dsem(b0)
        ins = fn(self.e[eng])
        b0.dcnt += 16
        ins.then_inc(sem, 16)
        tok = (sem, b0.dcnt)
        self._commit(tok, reads, writes)
        self.ninst += 1
        return tok

    def barrier(self):
        toks = [(self.sem[k], self.cnt[k]) for k in self.ENG if self.cnt[k] > 0]
        for b in self.dma_bufs:
            if b.dsem is not None and b.dcnt > 0 and b not in self.skip:
                toks.append((b.dsem, b.dcnt))
        for k in self.ENG:
            for t in toks:
                self._wait(k, t)


class Rot:
    def __init__(self, bufs):
        self.bufs = bufs
        self.i = 0

    def get(self):
        b = self.bufs[self.i % len(self.bufs)]
        self.i += 1
        return b


def host_consts(nxt):
    ntok = 128 * (2 + nxt)
    k = np.arange(128)
    U = (k[:, None] <= k[None, :]).astype(np.float32)
    Lst = (k[:, None] > k[None, :]).astype(np.float32)
    Lo = (k[:, None] >= k[None, :]).astype(np.float32)
    Ust = (k[:, None] < k[None, :]).astype(np.float32)
    ident = np.eye(128, dtype=np.float32)
    ones = np.ones((128, 128), np.float32)
    blk = np.zeros((128, 128), np.float32)
    blk[:64, :64] = 1
    blk[64:, 64:] = 1
    cm = np.concatenate([ident, U, Lst, Lo, Ust, ones, blk], axis=1)
    s = np.arange(128 * nxt)
    row = (s // 64).astype(np.float32)
    col = (s % 64).astype(np.float32)
    nq = 16
    inv = (10000.0 ** (-np.arange(nq, dtype=np.float32) / nq)).astype(np.float32)
    ar = row[:, None] * inv
    ac = col[:, None] * inv
    ang = np.concatenate([ar, ar, ac, ac], axis=-1)
    cos = np.cos(ang).astype(np.float32)
    sin = np.sin(ang).astype(np.float32)
    cosT = np.ones((128, ntok), np.float32)
    sinT = np.zeros((128, ntok), np.float32)
    cosT[:, 256:] = np.concatenate([cos.T, cos.T], axis=0)
    sinT[:, 256:] = np.concatenate([(sin * SIGN[None, :]).T, (sin * SIGN[None, :]).T], axis=0)
    return cm, cosT, sinT


CM_ID, CM_U, CM_LST, CM_LO, CM_UST, CM_ONES, CM_BLK = [i * 128 for i in range(7)]


def host_prep(inp, L, nxt):
    f = np.float32
    fmc = fm_colmap()
    tmc = tm_colmap()
    w_in = np.asarray(inp["w_in"], f)
    wfm = np.empty((L, NFMG, 128, 8, 512), f)
    wtm = np.empty((L, 128, 8, TMW), f)
    for l in range(L):
        g = w_in[l][:, fmc].reshape(8, 128, NFMG, 512)
        wfm[l] = g.transpose(2, 1, 0, 3)
        wtm[l] = w_in[l][:, tmc].reshape(8, 128, TMW).transpose(1, 0, 2)
    pp = np.zeros((L, 128, NPP), f)
    fp = np.zeros((L, 1, NFP), f)
    p64 = np.arange(128) % 64
    for l in range(L):
        pp[l, :, 0] = inp["b_qnorm"][l][p64]
        pp[l, :, 1] = inp["b_qnorm"][l][ROTP[p64]]
        pp[l, :, 2] = inp["b_knorm"][l][p64]
        pp[l, :, 3] = inp["b_knorm"][l][ROTP[p64]]
        cw = np.asarray(inp["m_conv_w"][l], f)
        for j in range(3):
            pp[l, :, 4 + 12 * j: 16 + 12 * j] = cw[j].reshape(12, 128).T
        pp[l, :, 40:52] = np.asarray(inp["m_conv_b"][l], f).reshape(12, 128).T
        pp[l, :, 52:60] = np.asarray(inp["g_norm1"][l], f).reshape(8, 128).T
        pp[l, :, 60:68] = np.asarray(inp["g_norm2"][l], f).reshape(8, 128).T
        pp[l, :, 68:116] = np.asarray(inp["b_ada"][l], f).reshape(48, 128).T
        pp[l, :, 116] = inp["c_subln"][l]
        fpl = fp[l, 0]
        fpl[0:8] = inp["a_sink"][l]
        fpl[8:72] = inp["c_lam_q1"][l]
        fpl[72:136] = inp["c_lam_k1"][l]
        fpl[136:200] = inp["c_lam_q2"][l]
        fpl[200:264] = inp["c_lam_k2"][l]
        fpl[264:392] = inp["c_subln"][l]
        fpl[392:424] = np.asarray(inp["m_dt_bias"][l], f).reshape(32)
        fpl[424:456] = np.asarray(inp["m_a_log"][l], f).reshape(32)
        fpl[456:472] = inp["m_d"][l]
        fpl[472:1496] = inp["m_norm"][l]
    gfin = np.asarray(inp["g_final"], f).reshape(8, 128).T.copy()
    skt = np.ascontiguousarray(np.asarray(inp["p_subkeys"], f).reshape(L, 16, 128, 128).transpose(0, 3, 1, 2))
    shared = {
        "wfm": wfm, "wtm": wtm, "pp": pp, "fp": fp, "gfin": gfin, "skt": skt,
        "w_ada": np.ascontiguousarray(inp["w_ada"], f), "w_br": np.ascontiguousarray(inp["w_br"], f),
        "w_out": np.ascontiguousarray(inp["w_out"], f), "p_wq": np.ascontiguousarray(inp["p_wq"], f),
        "p_u": np.ascontiguousarray(inp["p_u"], f), "p_v": np.ascontiguousarray(inp["p_v"], f),
    }
    cm, cosT, sinT = host_consts(nxt)
    shared.update({"cm": cm, "cosT": cosT, "sinT": sinT})
    return shared


def build(L, nxt, debug=False, stop=None, lam_inits=None):
    NT = 2 + nxt
    ntok = 128 * NT
    groups = [(0, 256)] + [(256 + 512 * i, 512) for i in range(nxt // 4)]
    nc = bass.Bass("TRN2", target_bir_lowering=False)
    k = MK(nc)

    def din(name, shape, dt=F32):
        return nc.dram_tensor(name, list(shape), dt, kind="ExternalInput").ap()

    def scr(name, shape, dt):
        return nc.dram_tensor(name, list(shape), dt, kind="ExternalOutput" if debug else "Internal").ap()

    x_in = din("x", [128 * nxt, D])
    ctx_in = din("ctx", [256, D])
    cvec = din("cvec", [128, 8, 2])
    wfm = din("wfm", [L, NFMG, 128, 8, 512])
    wtm = din("wtm", [L, 128, 8, TMW])
    pp_in = din("pp", [L, 128, NPP])
    fp_in = din("fp", [L, 1, NFP])
    gfin_in = din("gfin", [128, 8])
    skt_in = din("skt", [L, 128, 16, 128])
    w_ada = din("w_ada", [L, D, 6 * D])
    w_br = din("w_br", [L, 2560, D])
    w_out = din("w_out", [L, D, D])
    p_wq = din("p_wq", [L, D, 2048])
    p_u = din("p_u", [L, 16384, D])
    p_v = din("p_v", [L, 16384, D])
    cm_in = din("cm", [128, 896])
    cos_in = din("cosT", [128, ntok])
    sin_in = din("sinT", [128, ntok])
    out = nc.dram_tensor("out", [128 * nxt, D], F32, kind="ExternalOutput").ap()
    XT = scr("XT", [8, 128, ntok], F32)
    QK = scr("QK", [20, 128, ntok], BF16)
    VAUG = scr("VAUG", [NT, 128, VW], BF16)
    XTOK = scr("XTOK", [NT, 128, 1024], BF16)
    BTOK = scr("BTOK", [NT, 128, 256], BF16)
    BT = scr("BT", [2, 128, ntok], BF16)
    CT = scr("CT", [2, 128, ntok], BF16)
    ZS = scr("ZS", [NT, 128, 1024], BF16)
    DT = scr("DT", [NT, 128, 32], F32)
    GT = scr("GT", [32, 128, ntok], BF16)
    OT = scr("OT", [20, 128, ntok], BF16)
    YF = scr("YF", [NT, 128, 1024], F32)
    TT = scr("TT", [8, 128, ntok], BF16)
    UV = scr("UV", [16384, 2 * D], BF16)
    MODS = scr("MODS", [128, 48, 2], F32) if debug else None

    top = ExitStack()
    cm = k.sb(top, "cm", [128, 896], F32)
    ps_all = top.enter_context(nc.psum_tensor("ps", [128, 8, 512], F32))
    banks = [Buf(ps_all[:, i, :], "bank%d" % i) for i in range(8)]
    bank2 = [Buf(ps_all[:, 2 * i:2 * i + 2, :], "bank2_%d" % i) for i in range(4)]
    k.dma("sp", lambda e: e.dma_start(out=cm[:], in_=cm_in), writes=[cm])

    def C(off, n=128):
        return cm[:, off:off + n]

    ident = C(CM_ID)

    def done():
        k.barrier()
        print("MK: ninst", k.ninst, "nwaits", k.nwaits, "dsems", len(k.dsems), flush=True)
        top.close()
        k.es.close()
        return nc

    with ExitStack() as es:
        xin = Rot([k.sb(es, "xin%d" % i, [128, D], F32) for i in range(2)])
        xo = Rot([k.sb(es, "xo%d" % i, [128, 8, 128], F32) for i in range(2)])
        bk = Rot(banks)
        for t in range(NT):
            src = ctx_in[t * 128:(t + 1) * 128, :] if t < 2 else x_in[(t - 2) * 128:(t - 1) * 128, :]
            xi = xin.get()
            k.dma("sp", lambda e: e.dma_start(out=xi[:], in_=src), writes=[xi])
            xob = xo.get()
            for half in range(2):
                b = bk.get()

                def tr(e):
                    for c in range(4):
                        ins = e.transpose(b[:, c * 128:(c + 1) * 128], xi[:, (half * 4 + c) * 128:(half * 4 + c + 1) * 128], ident)
                    return ins
                k.op("pe", tr, reads=[xi, cm], writes=[b])
                k.op("act" if half else "dve", lambda e: (e.copy if half else e.tensor_copy)(
                    xob[:, half * 4:half * 4 + 4, :], b[:].rearrange("p (c t) -> p c t", c=4)), reads=[b], writes=[xob])
            k.dma("sp", lambda e: e.dma_start(out=XT[:, :, t * 128:(t + 1) * 128].rearrange("c p t -> p c t"), in_=xob[:]), reads=[xob])
        k.barrier()
    if stop == "I":
        return done()

    for l in range(L):
        lam_init = 0.8 - 0.6 * math.exp(-0.3 * l)
        lay = ExitStack()
        pp = k.sb(lay, "pp", [128, NPP], F32)
        fpb = k.sb(lay, "fpb", [128, NFP], F32)
        k.dma("sp", lambda e: e.dma_start(out=pp[:], in_=pp_in[l]), writes=[pp])
        k.dma("sp", lambda e: e.dma_start(out=fpb[:], in_=fp_in[l].partition_broadcast(128)), writes=[fpb])
        mods = k.sb(lay, "mods", [128, 48, 2], F32)
        gs1 = k.sb(lay, "gs1", [128, 8, 2], F32)
        gs2 = k.sb(lay, "gs2", [128, 8, 2], F32)

        with ExitStack() as es:
            sc = k.sb(es, "sc", [128, 8, 2], F32)
            k.dma("sp", lambda e: e.dma_start(out=sc[:], in_=cvec), writes=[sc])
            k.op("act", lambda e: e.activation(sc[:], sc[:], AF.Silu), reads=[sc], writes=[sc])
            was = Rot([k.sb(es, "wa%d" % i, [128, 8, 768], F32) for i in range(2)])
            mb = banks[0]
            for cg in range(8):
                wa = was.get()
                k.dma("sp" if cg % 2 else "act", lambda e: e.dma_start(
                    out=wa[:], in_=w_ada[l].rearrange("(k p) c -> p k c", p=128)[:, :, cg * 768:(cg + 1) * 768]), writes=[wa])

                def mm(e):
                    for j in range(6):
                        ch = cg * 6 + j
                        for kk in range(8):
                            ins = e.matmul(mb[:, ch * 2:ch * 2 + 2], wa[:, kk, j * 128:(j + 1) * 128], sc[:, kk, :],
                                           start=(kk == 0), stop=(kk == 7), skip_group_check=True)
                    return ins
                k.op("pe", mm, reads=[wa, sc], writes=[mb])
            k.op("dve", lambda e: e.tensor_tensor(mods[:], mb[:, 0:96].rearrange("p (c w) -> p c w", w=2),
                                                  pp[:, 68:116].unsqueeze(2).to_broadcast([128, 48, 2]), ALU.add),
                 reads=[mb, pp], writes=[mods])
            for (gs, part, gcol) in ((gs1, 1, 52), (gs2, 4, 60)):
                k.op("dve", lambda e: e.tensor_scalar(gs[:], mods[:, part * 8:part * 8 + 8, :], 1.0, None, ALU.add),
                     reads=[mods], writes=[gs])
                k.op("dve", lambda e: e.tensor_tensor(gs[:], gs[:], pp[:, gcol:gcol + 8].unsqueeze(2).to_broadcast([128, 8, 2]), ALU.mult),
                     reads=[gs, pp], writes=[gs])
            if debug:
                k.dma("sp", lambda e: e.dma_start(out=MODS, in_=mods[:]), reads=[mods])
            k.barrier()
        if stop == "M":
            lay.close()
            return done()

        def modcol(part, c, w):
            return mods[:, part * 8 + c, w:w + 1]

        with ExitStack() as es:
            hT = k.sb(es, "hT", [128, 8, ntok], BF16)
            cosT = k.sb(es, "cosT", [128, ntok], F32)
            sinT = k.sb(es, "sinT", [128, ntok], F32)
            k.dma("sp", lambda e: e.dma_start(out=cosT[:], in_=cos_in), writes=[cosT])
            k.dma("sp", lambda e: e.dma_start(out=sinT[:], in_=sin_in), writes=[sinT])
            bk = Rot(banks)
            with ExitStack() as es1:
                xg = Rot([k.sb(es1, "xg%d" % i, [128, 8, 512], F32) for i in range(2)])
                sq = k.sb(es1, "sq", [128, 8, 512], F32)
                rs = k.sb(es1, "rs", [128, 512], F32)
                tmp = Rot([k.sb(es1, "a1tmp%d" % i, [128, 512], F32) for i in range(2)])
                for (t0, n) in groups:
                    w = 1 if t0 == 0 else 0
                    x = xg.get()
                    k.dma("sp", lambda e: e.dma_start(out=x[:, :, 0:n], in_=XT[:, :, t0:t0 + n].rearrange("c p t -> p c t")), writes=[x])
                    k.op("act", lambda e: e.activation(sq[:, :, 0:n], x[:, :, 0:n], AF.Square), reads=[x], writes=[sq])
                    b = bk.get()

                    def mm(e):
                        for c in range(8):
                            ins = e.matmul(b[:, 0:n], C(CM_ONES), sq[:, c, 0:n], start=(c == 0), stop=(c == 7))
                        return ins
                    k.op("pe", mm, reads=[sq, cm], writes=[b])
                    k.op("dve", lambda e: e.tensor_scalar(rs[:, 0:n], b[:, 0:n], 1.0 / D, EPS, ALU.mult, ALU.add), reads=[b], writes=[rs])
                    k.op("act", lambda e: e.activation(rs[:, 0:n], rs[:, 0:n], AF.Sqrt), reads=[rs], writes=[rs])
                    k.op("dve", lambda e: e.reciprocal(rs[:, 0:n], rs[:, 0:n]), reads=[rs], writes=[rs])
                    for c in range(8):
                        tb = tmp.get()
                        k.op("dve", lambda e: e.scalar_tensor_tensor(tb[:, 0:n], x[:, c, 0:n], gs1[:, c, w:w + 1], rs[:, 0:n], ALU.mult, ALU.mult),
                             reads=[x, gs1, rs], writes=[tb])
                        k.op("act", lambda e: e.activation(hT[:, c, t0:t0 + n], tb[:, 0:n], AF.Identity, bias=modcol(0, c, w), scale=1.0),
                             reads=[tb, mods], writes=[hT])
                k.barrier()
            if stop == "A1":
                dbg = scr("HT", [128, 8, ntok], BF16)
                k.dma("sp", lambda e: e.dma_start(out=dbg, in_=hT[:]), reads=[hT])
                es.close(); lay.close()
                return done()
            with ExitStack() as es2:
                wgs = Rot([k.sb(es2, "wg%d" % i, [128, 8, 512], BF16) for i in range(2)])
                t1s = Rot([k.sb(es2, "t1_%d" % i, [128, 512], F32) for i in range(2)])
                t2s = Rot([k.sb(es2, "t2_%d" % i, [128, 512], F32) for i in range(2)])
                obs = Rot([k.sb(es2, "ob%d" % i, [128, 512], BF16) for i in range(3)])
                sqb = k.sb(es2, "sqb", [128, 512], F32)
                rsb = k.sb(es2, "rsb", [128, 512], F32)
                xrow = k.sb(es2, "xrow", [128, ntok + 4], F32)
                crow = k.sb(es2, "crow", [128, ntok], F32)
                trb = Rot([k.sb(es2, "trb%d" % i, [128, 4, 128], BF16) for i in range(2)])
                k.op("pool", lambda e: e.memset(xrow[:], 0.0), writes=[xrow])
                nx = 128 * nxt
                XOFF = 259

                def rowcol(t0):
                    return 1 + t0 if t0 < 256 else XOFF + (t0 - 256)

                def mm_chunk(wg, j, t0, n):
                    b = bk.get()

                    def mm(e):
                        for kk in range(8):
                            ins = e.matmul(b[:, 0:n], wg[:, kk, j * 128:(j + 1) * 128], hT[:, kk, t0:t0 + n],
                                           start=(kk == 0), stop=(kk == 7))
                        return ins
                    k.op("pe", mm, reads=[wg, hT], writes=[b])
                    return b

                for g in range(NFMG):
                    wg = wgs.get()
                    k.dma("pool", lambda e: e.dma_start(out=wg[:], in_=wfm[l, g]), writes=[wg])
                    if g < 10:
                        isB = g in (3, 4, 5)
                        for pi in range(2):
                            if g < 2:
                                qk = QK_AQ + g * 2 + pi
                            elif g == 2:
                                qk = QK_AK + pi
                            elif g < 5:
                                qk = QK_BQ + (g - 3) * 2 + pi
                            elif g == 5:
                                qk = QK_BK + pi
                            elif g < 8:
                                qk = QK_CQ + (g - 6) * 2 + pi
                            else:
                                qk = QK_CK + (g - 8) * 2 + pi
                            gcol = 0 if g in (3, 4) else 2
                            for (t0, n) in groups:
                                bp = mm_chunk(wg, 2 * pi, t0, n)
                                br = mm_chunk(wg, 2 * pi + 1, t0, n)
                                t1 = t1s.get()
                                t2 = t2s.get()
                                ob = obs.get()
                                if isB:
                                    k.op("act", lambda e: e.activation(sqb[:, 0:n], bp[:, 0:n], AF.Square), reads=[bp], writes=[sqb])
                                    bs = bk.get()
                                    k.op("pe", lambda e: e.matmul(bs[:, 0:n], C(CM_BLK), sqb[:, 0:n], start=True, stop=True),
                                         reads=[sqb, cm], writes=[bs])
                                    k.op("dve", lambda e: e.tensor_scalar(rsb[:, 0:n], bs[:, 0:n], 1.0 / HD, EPS, ALU.mult, ALU.add),
                                         reads=[bs], writes=[rsb])
                                    k.op("act", lambda e: e.activation(rsb[:, 0:n], rsb[:, 0:n], AF.Sqrt), reads=[rsb], writes=[rsb])
                                    k.op("dve", lambda e: e.reciprocal(rsb[:, 0:n], rsb[:, 0:n]), reads=[rsb], writes=[rsb])
                                    k.op("dve", lambda e: e.scalar_tensor_tensor(t1[:, 0:n], bp[:, 0:n], pp[:, gcol:gcol + 1], rsb[:, 0:n], ALU.mult, ALU.mult),
                                         reads=[bp, pp, rsb], writes=[t1])
                                    k.op("dve", lambda e: e.scalar_tensor_tensor(t2[:, 0:n], br[:, 0:n], pp[:, gcol + 1:gcol + 2], rsb[:, 0:n], ALU.mult, ALU.mult),
                                         reads=[br, pp, rsb], writes=[t2])
                                    k.op("pool", lambda e: e.tensor_tensor(t1[:, 0:n], t1[:, 0:n], cosT[:, t0:t0 + n], ALU.mult), reads=[t1, cosT], writes=[t1])
                                    k.op("pool", lambda e: e.tensor_tensor(t2[:, 0:n], t2[:, 0:n], sinT[:, t0:t0 + n], ALU.mult), reads=[t2, sinT], writes=[t2])
                                else:
                                    k.op("dve", lambda e: e.tensor_tensor(t1[:, 0:n], bp[:, 0:n], cosT[:, t0:t0 + n], ALU.mult), reads=[bp, cosT], writes=[t1])
                                    k.op("dve", lambda e: e.tensor_tensor(t2[:, 0:n], br[:, 0:n], sinT[:, t0:t0 + n], ALU.mult), reads=[br, sinT], writes=[t2])
                                k.op("pool", lambda e: e.tensor_tensor(ob[:, 0:n], t1[:, 0:n], t2[:, 0:n], ALU.add), reads=[t1, t2], writes=[ob])
                                k.dma("sp", lambda e: e.dma_start(out=QK[qk, :, t0:t0 + n], in_=ob[:, 0:n]), reads=[ob])
                    elif g < 13:
                        for j in range(4):
                            c = (g - 10) * 4 + j
                            for (t0, n) in groups:
                                b = mm_chunk(wg, j, t0, n)
                                rc = rowcol(t0)
                                k.op("act", lambda e: e.copy(xrow[:, rc:rc + n], b[:, 0:n]), reads=[b], writes=[xrow])
                            for (s0, sn, o0) in ((1, 256, 0), (XOFF, nx, 256)):
                                k.op("act", lambda e: e.activation(crow[:, o0:o0 + sn], xrow[:, s0 - 1:s0 - 1 + sn], AF.Identity,
                                                                   bias=pp[:, 40 + c:41 + c], scale=pp[:, 4 + c:5 + c]),
                                     reads=[xrow, pp], writes=[crow])
                                k.op("dve", lambda e: e.scalar_tensor_tensor(crow[:, o0:o0 + sn], xrow[:, s0:s0 + sn], pp[:, 16 + c:17 + c],
                                                                             crow[:, o0:o0 + sn], ALU.mult, ALU.add), reads=[xrow, pp, crow], writes=[crow])
                                k.op("dve", lambda e: e.scalar_tensor_tensor(crow[:, o0:o0 + sn], xrow[:, s0 + 1:s0 + 1 + sn], pp[:, 28 + c:29 + c],
                                                                             crow[:, o0:o0 + sn], ALU.mult, ALU.add), reads=[xrow, pp, crow], writes=[crow])
                            k.op("act", lambda e: e.activation(crow[:], crow[:], AF.Silu), reads=[crow], writes=[crow])
                            if c >= 8:
                                dst = BT if c < 10 else CT
                                gi = (c - 8) % 2
                                for (t0, n) in groups:
                                    ob = obs.get()
                                    k.op("pool", lambda e: e.tensor_copy(ob[:, 0:n], crow[:, t0:t0 + n]), reads=[crow], writes=[ob])
                                    k.dma("sp", lambda e: e.dma_start(out=dst[gi, :, t0:t0 + n], in_=ob[:, 0:n]), reads=[ob])
                            if c < 10:
                                for tb0 in range(0, NT, 4):
                                    nt4 = min(4, NT - tb0)
                                    b = bk.get()

                                    def tr(e):
                                        for q in range(nt4):
                                            ins = e.transpose(b[:, q * 128:(q + 1) * 128], crow[:, (tb0 + q) * 128:(tb0 + q + 1) * 128], ident)
                                        return ins
                                    k.op("pe", tr, reads=[crow, cm], writes=[b])
                                    tbf = trb.get()
                                    k.op("dve", lambda e: e.tensor_copy(tbf[:, 0:nt4, :], b[:, 0:nt4 * 128].rearrange("p (q c) -> p q c", q=nt4)),
                                         reads=[b], writes=[tbf])
                                    if c < 8:
                                        dd = XTOK[tb0:tb0 + nt4, :, c * 128:(c + 1) * 128]
                                    else:
                                        dd = BTOK[tb0:tb0 + nt4, :, (c - 8) * 128:(c - 7) * 128]
                                    k.dma("sp", lambda e: e.dma_start(out=dd.rearrange("t p c -> p t c"), in_=tbf[:, 0:nt4, :]), reads=[tbf])
                    else:
                        for j in range(4):
                            gc = (g - 13) * 4 + j
                            for (t0, n) in groups:
                                b = mm_chunk(wg, j, t0, n)
                                ob = obs.get()
                                k.op("act", lambda e: e.activation(ob[:, 0:n], b[:, 0:n], AF.Sigmoid), reads=[b], writes=[ob])
                                k.dma("sp", lambda e: e.dma_start(out=GT[gc, :, t0:t0 + n], in_=ob[:, 0:n]), reads=[ob])
                k.barrier()
            with ExitStack() as es3:
                wt = k.sb(es3, "wt", [128, 8, TMW], BF16)
                k.dma("pool", lambda e: e.dma_start(out=wt[:], in_=wtm[l]), writes=[wt])
                vas = Rot([k.sb(es3, "va%d" % i, [128, VW], BF16) for i in range(2)])
                zss = Rot([k.sb(es3, "zs%d" % i, [128, 1024], BF16) for i in range(2)])
                dts = Rot([k.sb(es3, "dt%d" % i, [128, 32], F32) for i in range(2)])
                for va in vas.bufs:
                    k.op("pool", lambda e: e.memset(va[:], 1.0), writes=[va])
                blocks = ((0, 256), (256, 512), (768, 512), (1280, 512), (1792, 32))
                for t in range(NT):
                    bs = []
                    for (c0, cn) in blocks:
                        b = bk.get()

                        def mm(e):
                            for kk in range(8):
                                ins = e.matmul(b[:, 0:cn], hT[:, kk, t * 128:(t + 1) * 128], wt[:, kk, c0:c0 + cn],
                                               start=(kk == 0), stop=(kk == 7))
                            return ins
                        k.op("pe", mm, reads=[wt, hT], writes=[b])
                        bs.append(b)
                    va = vas.get()
                    zs = zss.get()
                    dtb = dts.get()
                    k.op("dve", lambda e: e.tensor_copy(va[:, 0:260].rearrange("p (h w) -> p h w", w=65)[:, :, 0:64],
                                                        bs[0][:, 0:256].rearrange("p (h w) -> p h w", w=64)), reads=[bs[0]], writes=[va])
                    k.op("dve", lambda e: e.tensor_copy(va[:, 260:776].rearrange("p (h w) -> p h w", w=129)[:, :, 0:128],
                                                        bs[1][:, 0:512].rearrange("p (h w) -> p h w", w=128)), reads=[bs[1]], writes=[va])
                    k.op("act", lambda e: e.activation(zs[:, 0:512], bs[2][:, 0:512], AF.Silu), reads=[bs[2]], writes=[zs])
                    k.op("act", lambda e: e.activation(zs[:, 512:1024], bs[3][:, 0:512], AF.Silu), reads=[bs[3]], writes=[zs])
                    k.op("dve", lambda e: e.tensor_tensor(dtb[:], bs[4][:, 0:32], fpb[:, 392:424], ALU.add), reads=[bs[4], fpb], writes=[dtb])
                    k.op("act", lambda e: e.activation(dtb[:], dtb[:], AF.Exp), reads=[dtb], writes=[dtb])
                    k.op("act", lambda e: e.activation(dtb[:], dtb[:], AF.Ln, bias=1.0, scale=1.0), reads=[dtb], writes=[dtb])
                    k.dma("sp", lambda e: e.dma_start(out=VAUG[t], in_=va[:]), reads=[va])
                    k.dma("sp", lambda e: e.dma_start(out=ZS[t], in_=zs[:]), reads=[zs])
                    k.dma("sp", lambda e: e.dma_start(out=DT[t], in_=dtb[:]), reads=[dtb])
                k.barrier()
        if stop == "A":
            lay.close()
            return done()

        dd = Buf(None, "dramdummy")
        k.skip.append(dd)
        for tab_in, c0 in ((p_u, 0), (p_v, D)):
            for i in range(16):
                k.dma("pool", lambda e: e.dma_start(out=UV[i * 1024:(i + 1) * 1024, c0:c0 + D], in_=tab_in[l, i * 1024:(i + 1) * 1024, :]), writes=[dd])
        with ExitStack() as es:
            kt = k.sb(es, "kt", [128, 8, ntok], BF16)
            va = k.sb(es, "vall", [128, NT, VW], BF16)
            for i, qi_ in enumerate((QK_AK, QK_AK + 1, QK_BK, QK_BK + 1, QK_CK, QK_CK + 1, QK_CK + 2, QK_CK + 3)):
                k.dma("sp", lambda e: e.dma_start(out=kt[:, i, :], in_=QK[qi_]), writes=[kt])
            k.dma("sp", lambda e: e.dma_start(out=va[:], in_=VAUG.rearrange("t p w -> p t w")), writes=[va])
            small = k.sb(es, "attsmall", [128, 16], F32)
            lprod = k.sb(es, "lprod", [128, 64], F32)
            subs = k.sb(es, "subs", [128, 128], F32)
            k.op("act", lambda e: e.activation(small[:, 0:8], fpb[:, 0:8], AF.Exp), reads=[fpb], writes=[small])
            for j, (a0, b0) in enumerate(((8, 72), (136, 200))):
                k.op("dve", lambda e: e.tensor_tensor(lprod[:], fpb[:, a0:a0 + 64], fpb[:, b0:b0 + 64], ALU.mult), reads=[fpb], writes=[lprod])
                k.op("dve", lambda e: e.tensor_reduce(small[:, 9 + j:10 + j], lprod[:], AX.X, ALU.add), reads=[lprod], writes=[small])
            k.op("act", lambda e: e.activation(small[:, 9:11], small[:, 9:11], AF.Exp), reads=[small], writes=[small])
            k.op("dve", lambda e: e.tensor_tensor(small[:, 8:9], small[:, 10:11], small[:, 9:10], ALU.subtract), reads=[small], writes=[small])
            k.op("dve", lambda e: e.tensor_scalar(small[:, 8:9], small[:, 8:9], -lam_init, None, ALU.add), reads=[small], writes=[small])
            k.op("dve", lambda e: e.tensor_scalar(subs[:], fpb[:, 264:392], 1.0 - lam_init, None, ALU.mult), reads=[fpb], writes=[subs])
            subcol = k.sb(es, "subcol", [128, 1], F32)
            k.op("dve", lambda e: e.tensor_scalar(subcol[:], pp[:, 116:117], 1.0 - lam_init, None, ALU.mult), reads=[pp], writes=[subcol])
            bcf = Rot([k.sb(es, "bcf%d" % i, [128, 512], F32) for i in range(4)])
            dfs = Rot([k.sb(es, "dfs%d" % i, [128, 512], F32) for i in range(2)])
            DENS = (banks[7], banks[3])

            qgs = Rot([k.sb(es, "qg%d" % i, [128, 4, 512], BF16) for i in range(2)])
            pTs = Rot([k.sb(es, "pT%d" % i, [128, 512], BF16) for i in range(4)])
            otoks = Rot([k.sb(es, "otok%d" % i, [128, 4, 512], F32) for i in range(2)])
            oTs = Rot([k.sb(es, "oT%d" % i, [128, 512], BF16) for i in range(4)])
            rrs = Rot([k.sb(es, "rr%d" % i, [128, 2, 2], F32) for i in range(4)])
            dts_ = Rot([k.sb(es, "dtmp%d" % i, [128, 128], F32) for i in range(2)])
            junk = k.sb(es, "junk", [128, 128], F32)
            sss = Rot([k.sb(es, "ss%d" % i, [128, 1], F32) for i in range(4)])
            Srot = Rot(banks[0:3])
            Orot = Rot(bank2[2:4])
            O1rot = Rot(banks[4:7])
            BCb = banks[7]
            rdens = Rot([k.sb(es, "rden%d" % i, [128, 512], F32) for i in range(4)])
            bcss = Rot([k.sb(es, "bcs%d" % i, [64, 512], F32) for i in range(2)])

            for mixer in range(3):
                k.barrier()
                if mixer == 0:
                    qgroups = [[t] for t in range(NT)]
                else:
                    qgroups = [[0, 1]] + [[2 + 4 * i + j for j in range(4)] for i in range(nxt // 4)]
                qbase = (QK_AQ, QK_BQ, QK_CQ)[mixer]
                for qt in qgroups:
                    nq = len(qt)
                    n = nq * 128
                    q0 = qt[0] * 128
                    is_ctx = qt[0] < 2
                    qg = qgs.get()
                    for c in range(4):
                        k.dma("sp" if c % 2 else "act", lambda e: e.dma_start(out=qg[:, c, 0:n], in_=QK[qbase + c, :, q0:q0 + n]), writes=[qg])
                    if is_ctx:
                        keys = [(0, None), (1, None)]
                    elif mixer == 0:
                        t = qt[0]
                        keys = [(0, None), (1, None)]
                        if t - 1 >= 2:
                            keys.append((t - 1, CM_LO))
                        keys.append((t, None))
                        if t + 1 < NT:
                            keys.append((t + 1, CM_U))
                    else:
                        keys = [(j, None) for j in range(NT)]
                    otok = otoks.get()
                    Oprev = None
                    O = None
                    nk = len(keys)
                    steps = [(hd, ji) for hd in range(8) for ji in range(nk)]

                    def headcfg(hd):
                        if mixer == 0:
                            return hd // 4, (hd // 4) * 65, 65
                        elif mixer == 1:
                            return 2 + hd // 4, 130 + (hd // 4) * 65, 65
                        return 4 + hd // 2, 260 + (hd // 2) * 129, 129

                    def emitS(idx):
                        hd, ji = steps[idx]
                        kc = headcfg(hd)[0]
                        kti = keys[ji][0]
                        ps_ = slice((hd % 2) * 64, (hd % 2) * 64 + 64)
                        qc = hd // 2
                        sbk = Srot.get()
                        k.op("pe", lambda e: e.matmul(sbk[:, 0:n], kt[ps_, kc, kti * 128:(kti + 1) * 128], qg[ps_, qc, 0:n], start=True, stop=True),
                             reads=[kt, qg], writes=[sbk])
                        return sbk
                    LA = 2
                    Sq = [emitS(i) for i in range(min(LA, len(steps)))]
                    for idx, (hd, ji) in enumerate(steps):
                        half = hd % 2
                        kc, voff, vw = headcfg(hd)
                        kti, msk = keys[ji]
                        sbk = Sq.pop(0)
                        if idx + LA < len(steps):
                            Sq.append(emitS(idx + LA))
                        if ji == 0:
                            Oprev = O
                            O = Orot.get() if mixer == 0 else O1rot.get()
                        pT = pTs.get()
                        k.op("act", lambda e: e.activation(pT[:, 0:n], sbk[:, 0:n], AF.Exp, scale=0.125), reads=[sbk], writes=[pT])
                        if msk is not None:
                            k.op("dve", lambda e: e.tensor_tensor(pT[:, 0:n], pT[:, 0:n], C(msk), ALU.mult), reads=[pT, cm], writes=[pT])

                        def pv(e):
                            for qi in range(nq):
                                ins = e.matmul(O[:, qi // 2, (qi % 2) * 129:(qi % 2) * 129 + vw], pT[:, qi * 128:(qi + 1) * 128],
                                               va[:, kti, voff:voff + vw], start=(ji == 0 and qi % 2 == 0), stop=(ji == nk - 1),
                                               skip_group_check=True)
                            return ins
                        if mixer == 0:
                            k.op("pe", pv, reads=[pT, va], writes=[O])
                        elif mixer == 2:
                            DEN = DENS[half]
                            k.op("pe", lambda e: e.matmul(O[:, 0:n], va[:, kti, voff:voff + 128], pT[:, 0:n], start=(ji == 0), stop=(ji == nk - 1)),
                                 reads=[pT, va], writes=[O])
                            k.op("pe", lambda e: e.matmul(DEN[0:1, 0:n], va[:, kti, voff + 128:voff + 129], pT[:, 0:n], start=(ji == 0), stop=(ji == nk - 1),
                                                          skip_group_check=True), reads=[pT, va], writes=[DEN])
                            if ji == nk - 1 and half == 1:
                                O0, O1 = Oprev, O
                                hh = hd // 2
                                rd = rdens.get()
                                rd1 = rdens.get()
                                k.op("dve", lambda e: e.reciprocal(rd[0:1, 0:n], DENS[0][0:1, 0:n]), reads=[DENS[0]], writes=[rd])
                                k.op("dve", lambda e: e.reciprocal(rd1[0:1, 0:n], DENS[1][0:1, 0:n]), reads=[DENS[1]], writes=[rd1])
                                k.op("dve", lambda e: e.tensor_scalar(rd1[0:1, 0:n], rd1[0:1, 0:n], small[0:1, 8:9], None, ALU.mult), reads=[rd1, small], writes=[rd1])
                                BC0 = DENS[0]
                                BC1 = DENS[1]
                                k.op("pe", lambda e: e.matmul(BC0[:, 0:n], cm.t[0:1, CM_ONES:CM_ONES + 128], rd[0:1, 0:n], start=True, stop=True), reads=[rd, cm], writes=[BC0])
                                k.op("pe", lambda e: e.matmul(BC1[:, 0:n], cm.t[0:1, CM_ONES:CM_ONES + 128], rd1[0:1, 0:n], start=True, stop=True), reads=[rd1, cm], writes=[BC1])
                                bc0 = bcf.get()
                                bc1 = bcf.get()
                                dd_ = dfs.get()
                                k.op("act", lambda e: e.copy(bc0[:, 0:n], BC0[:, 0:n]), reads=[BC0], writes=[bc0])
                                k.op("act", lambda e: e.copy(bc1[:, 0:n], BC1[:, 0:n]), reads=[BC1], writes=[bc1])
                                k.op("dve", lambda e: e.tensor_tensor(dd_[:, 0:n], O0[:, 0:n], bc0[:, 0:n], ALU.mult), reads=[O0, bc0], writes=[dd_])
                                k.op("dve", lambda e: e.tensor_tensor(bc1[:, 0:n], O1[:, 0:n], bc1[:, 0:n], ALU.mult), reads=[O1, bc1], writes=[bc1])
                                k.op("pool", lambda e: e.tensor_tensor(dd_[:, 0:n], dd_[:, 0:n], bc1[:, 0:n], ALU.add), reads=[dd_, bc1], writes=[dd_])
                                k.op("pool", lambda e: e.tensor_tensor(bc0[:, 0:n], dd_[:, 0:n], dd_[:, 0:n], ALU.mult), reads=[dd_], writes=[bc0])
                                SSb = O1rot.get()
                                k.op("pe", lambda e: e.matmul(SSb[:, 0:n], C(CM_ONES), bc0[:, 0:n], start=True, stop=True), reads=[bc0, cm], writes=[SSb])
                                k.op("dve", lambda e: e.tensor_scalar(bc1[:, 0:n], SSb[:, 0:n], 1.0 / 128, EPS, ALU.mult, ALU.add), reads=[SSb], writes=[bc1])
                                k.op("act", lambda e: e.activation(bc1[:, 0:n], bc1[:, 0:n], AF.Ln), reads=[bc1], writes=[bc1])
                                k.op("act", lambda e: e.activation(bc1[:, 0:n], bc1[:, 0:n], AF.Exp, scale=-0.5), reads=[bc1], writes=[bc1])
                                oT = oTs.get()
                                k.op("dve", lambda e: e.scalar_tensor_tensor(oT[:, 0:n], dd_[:, 0:n], subcol[:, 0:1], bc1[:, 0:n], ALU.mult, ALU.mult),
                                     reads=[dd_, subcol, bc1], writes=[oT])
                                k.dma("sp", lambda e: e.dma_start(out=OT[8 + hh, :, q0:q0 + n], in_=oT[:, 0:n]), reads=[oT])
                            continue
                        else:
                            k.op("pe", lambda e: e.matmul(O[0:65, 0:n], va[:, kti, voff:voff + 65], pT[:, 0:n], start=(ji == 0), stop=(ji == nk - 1)),
                                 reads=[pT, va], writes=[O])
                        if ji != nk - 1:
                            continue
                        if mixer == 1:
                            rd = rdens.get()
                            if mixer == 0:
                                k.op("dve", lambda e: e.tensor_scalar(rd[64:65, 0:n], O[64:65, 0:n], small[64:65, hd:hd + 1], None, ALU.add), reads=[O, small], writes=[rd])
                                k.op("dve", lambda e: e.reciprocal(rd[64:65, 0:n], rd[64:65, 0:n]), reads=[rd], writes=[rd])
                            else:
                                k.op("dve", lambda e: e.reciprocal(rd[64:65, 0:n], O[64:65, 0:n]), reads=[O], writes=[rd])
                            k.op("pe", lambda e: e.matmul(BCb[0:64, 0:n], cm.t[64:65, CM_ONES:CM_ONES + 64], rd[64:65, 0:n], start=True, stop=True),
                                 reads=[rd, cm], writes=[BCb])
                            bcs = bcss.get()
                            k.op("act", lambda e: e.copy(bcs[0:64, 0:n], BCb[0:64, 0:n]), reads=[BCb], writes=[bcs])
                            oT = oTs.get()
                            k.op("dve", lambda e: e.tensor_tensor(oT[0:64, 0:n], O[0:64, 0:n], bcs[0:64, 0:n], ALU.mult), reads=[O, bcs], writes=[oT])
                            k.dma("sp", lambda e: e.dma_start(out=OT[mixer * 4 + hd // 2, (hd % 2) * 64:(hd % 2) * 64 + 64, q0:q0 + n], in_=oT[0:64, 0:n]), reads=[oT])
                            continue
                        nb = (nq + 1) // 2
                        npos = min(nq, 2)
                        dcol = vw - 1
                        if mixer < 2:
                            rr = rrs.get()
                            den = O[:, 0:nb, dcol:dcol + 129 * (npos - 1) + 1:129]
                            if mixer == 0:
                                k.op("dve", lambda e: e.tensor_scalar(rr[:, 0:nb, 0:npos], den, small[:, hd:hd + 1], None, ALU.add), reads=[O, small], writes=[rr])
                                k.op("dve", lambda e: e.reciprocal(rr[:, 0:nb, 0:npos], rr[:, 0:nb, 0:npos]), reads=[rr], writes=[rr])
                            else:
                                k.op("dve", lambda e: e.reciprocal(rr[:, 0:nb, 0:npos], den), reads=[O], writes=[rr])
                            for qi in range(nq):
                                k.op("dve", lambda e: e.tensor_scalar(otok[:, qi, hd * 64:(hd + 1) * 64], O[:, qi // 2, (qi % 2) * 129:(qi % 2) * 129 + 64],
                                                                      rr[:, qi // 2, qi % 2:qi % 2 + 1], None, ALU.mult), reads=[O, rr], writes=[otok])
                        elif half == 1:
                            O0, O1 = Oprev, O
                            r0 = rrs.get()
                            r1 = rrs.get()
                            k.op("dve", lambda e: e.reciprocal(r0[:, 0:nb, 0:npos], O0[:, 0:nb, dcol:dcol + 129 * (npos - 1) + 1:129]), reads=[O0], writes=[r0])
                            k.op("dve", lambda e: e.reciprocal(r1[:, 0:nb, 0:npos], O1[:, 0:nb, dcol:dcol + 129 * (npos - 1) + 1:129]), reads=[O1], writes=[r1])
                            k.op("dve", lambda e: e.tensor_scalar(r1[:, 0:nb, 0:npos], r1[:, 0:nb, 0:npos], small[:, 8:9], None, ALU.mult), reads=[r1, small], writes=[r1])
                            hh = hd // 2
                            for qi in range(nq):
                                dtm = dts_.get()
                                ss = sss.get()
                                osl = slice((qi % 2) * 129, (qi % 2) * 129 + 128)
                                k.op("dve", lambda e: e.tensor_scalar(dtm[:], O0[:, qi // 2, osl], r0[:, qi // 2, qi % 2:qi % 2 + 1], None, ALU.mult),
                                     reads=[O0, r0], writes=[dtm])
                                k.op("dve", lambda e: e.scalar_tensor_tensor(dtm[:], O1[:, qi // 2, osl], r1[:, qi // 2, qi % 2:qi % 2 + 1], dtm[:], ALU.mult, ALU.add),
                                     reads=[O1, r1, dtm], writes=[dtm])
                                k.op("pool", lambda e: e.tensor_tensor(junk[:], dtm[:], dtm[:], ALU.mult), reads=[dtm], writes=[junk])
                                k.op("dve", lambda e: e.tensor_reduce(ss[:], junk[:], AX.X, ALU.add), reads=[junk], writes=[ss])
                                k.op("dve", lambda e: e.tensor_scalar(ss[:], ss[:], 1.0 / 128, EPS, ALU.mult, ALU.add), reads=[ss], writes=[ss])
                                k.op("act", lambda e: e.activation(ss[:], ss[:], AF.Ln), reads=[ss], writes=[ss])
                                k.op("act", lambda e: e.activation(ss[:], ss[:], AF.Exp, scale=-0.5), reads=[ss], writes=[ss])
                                k.op("dve", lambda e: e.scalar_tensor_tensor(otok[:, qi, hh * 128:(hh + 1) * 128], dtm[:], ss[:, 0:1], subs[:], ALU.mult, ALU.mult),
                                     reads=[dtm, ss, subs], writes=[otok])
                    for c in (range(4) if mixer == 0 else ()):
                        b = Srot.get()

                        def tr(e):
                            for qi in range(nq):
                                ins = e.transpose(b[:, qi * 128:(qi + 1) * 128], otok[:, qi, c * 128:(c + 1) * 128], ident)
                            return ins
                        k.op("pe", tr, reads=[otok, cm], writes=[b])
                        oT = oTs.get()
                        k.op("act", lambda e: e.copy(oT[:, 0:n], b[:, 0:n]), reads=[b], writes=[oT])
                        k.dma("sp", lambda e: e.dma_start(out=OT[mixer * 4 + c, :, q0:q0 + n], in_=oT[:, 0:n]), reads=[oT])
            k.barrier()
        if stop == "B":
            lay.close()
            return done()

        with ExitStack() as es:
            aneg = k.sb(es, "aneg", [128, 32], F32)
            k.op("act", lambda e: e.activation(aneg[:], fpb[:, 424:456], AF.Exp), reads=[fpb], writes=[aneg])
            k.op("dve", lambda e: e.tensor_scalar(aneg[:], aneg[:], -1.0, None, ALU.mult), reads=[aneg], writes=[aneg])
            Hf = k.sb(es, "Hf", [128, 2, 512], F32)
            Hb = k.sb(es, "Hb", [128, 2, 512], BF16)
            xts = Rot([k.sb(es, "xt%d" % i, [128, 1024], BF16) for i in range(3)])
            dtts = Rot([k.sb(es, "dtt%d" % i, [128, 32], F32) for i in range(3)])
            btoks = Rot([k.sb(es, "btk%d" % i, [128, 256], BF16) for i in range(3)])
            bTs = Rot([k.sb(es, "bT%d" % i, [128, 2, 128], BF16) for i in range(3)])
            cTs = Rot([k.sb(es, "cT%d" % i, [128, 2, 128], BF16) for i in range(3)])
            zss = Rot([k.sb(es, "zz%d" % i, [128, 1024], BF16) for i in range(2)])
            yfs = Rot([k.sb(es, "yf%d" % i, [128, 1024], F32) for i in range(2)])
            a_s = Rot([k.sb(es, "a%d" % i, [128, 16], F32) for i in range(2)])
            Es = Rot([k.sb(es, "E%d" % i, [128, 48], F32) for i in range(3)])
            Xds = Rot([k.sb(es, "Xd%d" % i, [128, 1024], BF16) for i in range(3)])
            Xss = Rot([k.sb(es, "Xs%d" % i, [128, 1024], BF16) for i in range(2)])
            cbms = Rot([k.sb(es, "cbm%d" % i, [128, 2, 128], F32) for i in range(2)])
            lts = Rot([k.sb(es, "lt%d" % i, [128, 16, 128], F32) for i in range(2)])
            Lms = Rot([k.sb(es, "Lm%d" % i, [128, 8, 128], F32) for i in range(2)])
            Mts = Rot([k.sb(es, "Mt%d" % i, [128, 8, 128], BF16) for i in range(4)])
            yos = Rot([k.sb(es, "yo%d" % i, [128, 1024], F32) for i in range(2)])
            ytots = Rot([k.sb(es, "ytot%d" % i, [128, 1024], F32) for i in range(2)])
            tmps = Rot([k.sb(es, "ytmp%d" % i, [128, 1024], F32) for i in range(2)])
            rst = Rot([k.sb(es, "rst%d" % i, [128, 2], F32) for i in range(2)])
            sTs = Rot([k.sb(es, "sT%d" % i, [128, 8, 128], BF16) for i in range(2)])
            R1 = Rot(banks[0:2])
            R2 = Rot(bank2[1:4])

            def v3(ap, a, b):
                return ap.rearrange("p (a b) -> p a b", a=a, b=b)

            def ssd_stage1(t, d):
                xt = xts.get(); dtt = dtts.get(); btk = btoks.get(); bT = bTs.get(); cT = cTs.get()
                k.dma("sp", lambda e: e.dma_start(out=xt[:], in_=XTOK[t]), writes=[xt])
                k.dma("sp", lambda e: e.dma_start(out=dtt[:], in_=DT[t]), writes=[dtt])
                k.dma("act", lambda e: e.dma_start(out=btk[:], in_=BTOK[t]), writes=[btk])
                k.dma("act", lambda e: e.dma_start(out=bT[:], in_=BT[:, :, t * 128:(t + 1) * 128].rearrange("g p t -> p g t")), writes=[bT])
                k.dma("act", lambda e: e.dma_start(out=cT[:], in_=CT[:, :, t * 128:(t + 1) * 128].rearrange("g p t -> p g t")), writes=[cT])
                dsl = slice(d * 16, d * 16 + 16)
                tri_i, tri_x, maskl, m01 = (CM_U, CM_LST, CM_LST, CM_U) if d == 0 else (CM_LO, CM_UST, CM_UST, CM_LO)
                a = a_s.get()
                k.op("dve", lambda e: e.tensor_tensor(a[:], dtt[:, dsl], aneg[:, dsl], ALU.mult), reads=[dtt, aneg], writes=[a])
                Xd = Xds.get()
                k.op("dve", lambda e: e.tensor_tensor(v3(Xd[:], 16, 64), v3(xt[:], 16, 64), dtt[:, dsl].unsqueeze(2).to_broadcast([128, 16, 64]), ALU.mult),
                     reads=[xt, dtt], writes=[Xd])
                pc_ = R1.get()

                def cums(e):
                    e.matmul(pc_[:, 0:16], C(tri_i), a[:], start=True, stop=True, skip_group_check=True)
                    e.matmul(pc_[:, 16:32], C(tri_x), a[:], start=False, stop=True, skip_group_check=True)
                    return e.matmul(pc_[:, 32:48], C(CM_ONES), a[:], start=False, stop=True, skip_group_check=True)
                k.op("pe", cums, reads=[a, cm], writes=[pc_])
                E = Es.get()
                k.op("act", lambda e: e.activation(E[:], pc_[:, 0:48], AF.Exp), reads=[pc_], writes=[E])
                pcb = R1.get()

                def cbmm(e):
                    e.matmul(pcb[:, 0:128], bT[:, 0, :], cT[:, 0, :], start=True, stop=True, skip_group_check=True)
                    return e.matmul(pcb[:, 128:256], bT[:, 1, :], cT[:, 1, :], start=False, stop=True, skip_group_check=True)
                k.op("pe", cbmm, reads=[bT, cT], writes=[pcb])
                cbm = cbms.get()
                k.op("dve", lambda e: e.tensor_tensor(cbm[:], v3(pcb[:, 0:256], 2, 128), C(m01).unsqueeze(1).to_broadcast([128, 2, 128]), ALU.mult),
                     reads=[pcb, cm], writes=[cbm])
                lt = lts.get()
                k.op("pool", lambda e: e.tensor_tensor(lt[:], C(maskl).unsqueeze(1).to_broadcast([128, 16, 128]),
                                                       a[:].unsqueeze(2).to_broadcast([128, 16, 128]), ALU.mult), reads=[a, cm], writes=[lt])
                Mtl = []
                for g in range(2):
                    pd = bank2[2]

                    def dmm(e):
                        for ee in range(8):
                            ins = e.matmul(pd[:, ee // 4, (ee % 4) * 128:(ee % 4 + 1) * 128], lt[:, g * 8 + ee, :], C(tri_i),
                                           start=(ee % 4 == 0), stop=True, skip_group_check=True)
                        return ins
                    k.op("pe", dmm, reads=[lt, cm], writes=[pd])
                    Lm = Lms.get()
                    for hb in range(2):
                        k.op("act", lambda e: e.activation(Lm[:, hb * 4:hb * 4 + 4, :], v3(pd[:, hb, :], 4, 128), AF.Exp), reads=[pd], writes=[Lm])
                    Mt = Mts.get()
                    k.op("dve", lambda e: e.tensor_tensor(Mt[:], Lm[:], cbm[:, g, :].unsqueeze(1).to_broadcast([128, 8, 128]), ALU.mult),
                         reads=[Lm, cbm], writes=[Mt])
                    Mtl.append(Mt)
                return dict(xt=xt, btk=btk, cT=cT, E=E, Xd=Xd, Mtl=Mtl)

            def ssd_stage2(t, d, finish, st):
                xt, btk, cT, E, Xd, Mtl = st["xt"], st["btk"], st["cT"], st["E"], st["Xd"], st["Mtl"]
                Y = bank2[1]
                for g in range(2):
                    Mt = Mtl[g]

                    def ymm(e):
                        for ee in range(8):
                            hh = g * 8 + ee
                            ins = e.matmul(Y[:, g, ee * 64:(ee + 1) * 64], Mt[:, ee, :], Xd[:, hh * 64:(hh + 1) * 64],
                                           start=(ee == 0), stop=True, skip_group_check=True)
                        return ins
                    k.op("pe", ymm, reads=[Mt, Xd], writes=[Y])
                Yo = bank2[3]

                def yomm(e):
                    e.matmul(Yo[:, 0, :], cT[:, 0, :], Hb[:, 0, :], start=True, stop=True, skip_group_check=True)
                    return e.matmul(Yo[:, 1, :], cT[:, 1, :], Hb[:, 1, :], start=True, stop=True, skip_group_check=True)
                k.op("pe", yomm, reads=[cT, Hb], writes=[Yo])
                yo = yos.get()
                k.op("dve", lambda e: e.tensor_tensor(v3(yo[:], 16, 64), Yo[:].rearrange("p g (e q) -> p (g e) q", q=64),
                                                      E[:, 0:16].unsqueeze(2).to_broadcast([128, 16, 64]), ALU.mult), reads=[Yo, E], writes=[yo])
                Xs = Xss.get()
                k.op("pool", lambda e: e.tensor_tensor(v3(Xs[:], 16, 64), v3(Xd[:], 16, 64), E[:, 16:32].unsqueeze(2).to_broadcast([128, 16, 64]), ALU.mult),
                     reads=[Xd, E], writes=[Xs])
                Hn = bank2[3]

                def hmm(e):
                    e.matmul(Hn[:, 0, :], btk[:, 0:128], Xs[:, 0:512], start=True, stop=True, skip_group_check=True)
                    return e.matmul(Hn[:, 1, :], btk[:, 128:256], Xs[:, 512:1024], start=True, stop=True, skip_group_check=True)
                k.op("pe", hmm, reads=[btk, Xs], writes=[Hn])
                k.op("dve", lambda e: e.tensor_tensor(Hf[:].rearrange("p g (e q) -> p (g e) q", q=64), Hf[:].rearrange("p g (e q) -> p (g e) q", q=64),
                                                      E[:, 32:48].unsqueeze(2).to_broadcast([128, 16, 64]), ALU.mult), reads=[Hf, E], writes=[Hf])
                k.op("dve", lambda e: e.tensor_tensor(Hf[:], Hf[:], Hn[:], ALU.add), reads=[Hf, Hn], writes=[Hf])
                k.op("pool", lambda e: e.tensor_copy(Hb[:], Hf[:]), reads=[Hf], writes=[Hb])
                if not finish:
                    yf = yfs.get()
                    k.op("dve", lambda e: e.tensor_tensor(yf[:], Y[:].rearrange("p g c -> p (g c)"), yo[:], ALU.add), reads=[Y, yo], writes=[yf])
                    k.dma("sp", lambda e: e.dma_start(out=YF[t], in_=yf[:]), reads=[yf])
                    return
                yf = yfs.get(); zz = zss.get()
                k.dma("sp", lambda e: e.dma_start(out=yf[:], in_=YF[t]), writes=[yf])
                k.dma("sp", lambda e: e.dma_start(out=zz[:], in_=ZS[t]), writes=[zz])
                yt = ytots.get(); tm = tmps.get()
                k.op("dve", lambda e: e.tensor_tensor(yt[:], Y[:].rearrange("p g c -> p (g c)"), yo[:], ALU.add), reads=[Y, yo], writes=[yt])
                k.op("pool", lambda e: e.tensor_tensor(yt[:], yt[:], yf[:], ALU.add), reads=[yt, yf], writes=[yt])
                k.op("pool", lambda e: e.tensor_tensor(v3(tm[:], 16, 64), v3(xt[:], 16, 64), fpb[:, 456:472].unsqueeze(2).to_broadcast([128, 16, 64]), ALU.mult),
                     reads=[xt, fpb], writes=[tm])
                k.op("pool", lambda e: e.tensor_tensor(yt[:], yt[:], tm[:], ALU.add), reads=[yt, tm], writes=[yt])
                k.op("dve", lambda e: e.tensor_tensor(yt[:], yt[:], zz[:], ALU.mult), reads=[yt, zz], writes=[yt])
                k.op("pool", lambda e: e.tensor_tensor(tm[:], yt[:], yt[:], ALU.mult), reads=[yt], writes=[tm])
                rs_ = rst.get()
                k.op("dve", lambda e: e.tensor_reduce(rs_[:], v3(tm[:], 2, 512), AX.X, ALU.add), reads=[tm], writes=[rs_])
                k.op("dve", lambda e: e.tensor_scalar(rs_[:], rs_[:], 1.0 / 512, EPS, ALU.mult, ALU.add), reads=[rs_], writes=[rs_])
                k.op("act", lambda e: e.activation(rs_[:], rs_[:], AF.Sqrt), reads=[rs_], writes=[rs_])
                k.op("dve", lambda e: e.reciprocal(rs_[:], rs_[:]), reads=[rs_], writes=[rs_])
                k.op("dve", lambda e: e.tensor_tensor(v3(yt[:], 2, 512), v3(yt[:], 2, 512), rs_[:].unsqueeze(2).to_broadcast([128, 2, 512]), ALU.mult),
                     reads=[yt, rs_], writes=[yt])
                k.op("pool", lambda e: e.tensor_tensor(yt[:], yt[:], fpb[:, 472:1496], ALU.mult), reads=[yt, fpb], writes=[yt])
                sT = sTs.get()
                for hb in range(2):
                    b = R1.get()

                    def tr(e):
                        for q in range(4):
                            cc = hb * 4 + q
                            ins = e.transpose(b[:, q * 128:(q + 1) * 128], yt[:, cc * 128:(cc + 1) * 128], ident)
                        return ins
                    k.op("pe", tr, reads=[yt, cm], writes=[b])
                    k.op("act", lambda e: e.copy(sT[:, hb * 4:hb * 4 + 4, :], v3(b[:], 4, 128)), reads=[b], writes=[sT])
                k.dma("sp", lambda e: e.dma_start(out=OT[12:20, :, t * 128:(t + 1) * 128].rearrange("c p t -> p c t"), in_=sT[:]), reads=[sT])

            for d in range(2):
                k.op("pool", lambda e: e.memset(Hf[:], 0.0), writes=[Hf])
                k.op("pool", lambda e: e.memset(Hb[:], 0.0), writes=[Hb])
                order = list(range(NT)) if d == 0 else [1, 0] + list(range(NT - 1, 1, -1))
                st = ssd_stage1(order[0], d)
                for i, t in enumerate(order):
                    nst = ssd_stage1(order[i + 1], d) if i + 1 < len(order) else None
                    ssd_stage2(t, d, d == 1, st)
                    st = nst
                k.barrier()
        if stop == "D":
            lay.close()
            return done()

        with ExitStack() as es:
            wbr = k.sb(es, "wbr", [128, 20, 1024], BF16)
            wo = k.sb(es, "wo", [128, 8, 1024], BF16)
            for i in range(4):
                k.dma("pool", lambda e: e.dma_start(out=wbr[:, i * 5:(i + 1) * 5, :], in_=w_br[l].rearrange("(f p) d -> p f d", p=128)[:, i * 5:(i + 1) * 5, :]), writes=[wbr])
            k.dma("pool", lambda e: e.dma_start(out=wo[:], in_=w_out[l].rearrange("(f p) d -> p f d", p=128)), writes=[wo])
            oTg = k.sb(es, "oTg", [128, 20, 512], BF16)
            gTg = k.sb(es, "gTg", [128, 32, 512], BF16)
            xTg = k.sb(es, "xTg", [128, 8, 512], F32)
            accf = k.sb(es, "accf", [128, 8, 512], F32)
            accb = k.sb(es, "accb", [128, 8, 512], BF16)
            tmpE = Rot([k.sb(es, "tmpE%d" % i, [128, 512], F32) for i in range(2)])
            sqE = accf
            rsE = k.sb(es, "rsE", [128, 512], F32)
            tTg = k.sb(es, "tTg", [128, 8, 512], BF16)
            bk = Rot(banks)
            brc = ((0, 4), (4, 8), (8, 12), (12, 20))
            for (t0, n) in groups:
                w = 1 if t0 == 0 else 0
                k.dma("sp", lambda e: e.dma_start(out=oTg[:, :, 0:n], in_=OT[:, :, t0:t0 + n].rearrange("c p t -> p c t")), writes=[oTg])
                k.dma("act", lambda e: e.dma_start(out=gTg[:, :, 0:n], in_=GT[:, :, t0:t0 + n].rearrange("c p t -> p c t")), writes=[gTg])
                k.dma("sp", lambda e: e.dma_start(out=xTg[:, :, 0:n], in_=XT[:, :, t0:t0 + n].rearrange("c p t -> p c t")), writes=[xTg])
                for dm in range(8):
                    for br in range(4):
                        b = bk.get()
                        f0, f1 = brc[br]

                        def mm(e):
                            for fc in range(f0, f1):
                                ins = e.matmul(b[:, 0:n], wbr[:, fc, dm * 128:(dm + 1) * 128], oTg[:, fc, 0:n], start=(fc == f0), stop=(fc == f1 - 1))
                            return ins
                        k.op("pe", mm, reads=[wbr, oTg], writes=[b])
                        if br == 0:
                            k.op("dve", lambda e: e.tensor_tensor(accf[:, dm, 0:n], b[:, 0:n], gTg[:, br * 8 + dm, 0:n], ALU.mult), reads=[b, gTg], writes=[accf])
                        else:
                            tb = tmpE.get()
                            k.op("dve", lambda e: e.tensor_tensor(tb[:, 0:n], b[:, 0:n], gTg[:, br * 8 + dm, 0:n], ALU.mult), reads=[b, gTg], writes=[tb])
                            if br < 3:
                                k.op("pool", lambda e: e.tensor_tensor(accf[:, dm, 0:n], accf[:, dm, 0:n], tb[:, 0:n], ALU.add), reads=[accf, tb], writes=[accf])
                            else:
                                k.op("pool", lambda e: e.tensor_tensor(accb[:, dm, 0:n], accf[:, dm, 0:n], tb[:, 0:n], ALU.add), reads=[accf, tb], writes=[accb])
                for dm2 in range(8):
                    b = bk.get()

                    def mm(e):
                        for dm in range(8):
                            ins = e.matmul(b[:, 0:n], wo[:, dm, dm2 * 128:(dm2 + 1) * 128], accb[:, dm, 0:n], start=(dm == 0), stop=(dm == 7))
                        return ins
                    k.op("pe", mm, reads=[wo, accb], writes=[b])
                    k.op("dve", lambda e: e.scalar_tensor_tensor(xTg[:, dm2, 0:n], b[:, 0:n], modcol(2, dm2, w), xTg[:, dm2, 0:n], ALU.mult, ALU.add),
                         reads=[b, mods, xTg], writes=[xTg])
                k.dma("sp", lambda e: e.dma_start(out=XT[:, :, t0:t0 + n].rearrange("c p t -> p c t"), in_=xTg[:, :, 0:n]), reads=[xTg])
                k.op("act", lambda e: e.activation(sqE[:, :, 0:n], xTg[:, :, 0:n], AF.Square), reads=[xTg], writes=[sqE])
                b = bk.get()

                def mm(e):
                    for c in range(8):
                        ins = e.matmul(b[:, 0:n], C(CM_ONES), sqE[:, c, 0:n], start=(c == 0), stop=(c == 7))
                    return ins
                k.op("pe", mm, reads=[sqE, cm], writes=[b])
                k.op("dve", lambda e: e.tensor_scalar(rsE[:, 0:n], b[:, 0:n], 1.0 / D, EPS, ALU.mult, ALU.add), reads=[b], writes=[rsE])
                k.op("act", lambda e: e.activation(rsE[:, 0:n], rsE[:, 0:n], AF.Sqrt), reads=[rsE], writes=[rsE])
                k.op("dve", lambda e: e.reciprocal(rsE[:, 0:n], rsE[:, 0:n]), reads=[rsE], writes=[rsE])
                for c in range(8):
                    tb = tmpE.get()
                    k.op("dve", lambda e: e.scalar_tensor_tensor(tb[:, 0:n], xTg[:, c, 0:n], gs2[:, c, w:w + 1], rsE[:, 0:n], ALU.mult, ALU.mult),
                         reads=[xTg, gs2, rsE], writes=[tb])
                    k.op("act", lambda e: e.activation(tTg[:, c, 0:n], tb[:, 0:n], AF.Identity, bias=modcol(3, c, w), scale=1.0),
                         reads=[tb, mods], writes=[tTg])
                k.dma("sp", lambda e: e.dma_start(out=TT[:, :, t0:t0 + n].rearrange("c p t -> p c t"), in_=tTg[:, :, 0:n]), reads=[tTg])
            k.barrier()
        if stop == "E":
            lay.close()
            return done()

        with ExitStack() as es:
            wq = k.sb(es, "wq", [128, 8, 2048], BF16)
            sk = k.sb(es, "sk", [128, 16, 128], BF16)
            for i in range(2):
                k.dma("pool", lambda e: e.dma_start(out=wq[:, i * 4:(i + 1) * 4, :], in_=p_wq[l].rearrange("(f p) d -> p f d", p=128)[:, i * 4:(i + 1) * 4, :]), writes=[wq])
            k.dma("pool", lambda e: e.dma_start(out=sk[:], in_=skt_in[l]), writes=[sk])
            identb = k.sb(es, "identb", [128, 128], BF16)
            iota16 = k.sb(es, "iota16", [128, 16], F32)
            k.op("dve", lambda e: e.tensor_copy(identb[:], ident), reads=[cm], writes=[identb])
            for i in range(16):
                k.op("dve", lambda e: e.memset(iota16[:, i:i + 1], float(i)), writes=[iota16])
            tTs = Rot([k.sb(es, "tT%d" % i, [128, 8, 128], BF16) for i in range(2)])
            tTf = k.sb(es, "tTf", [128, 8, 128], F32)
            ttoks = Rot([k.sb(es, "ttok%d" % i, [128, 1024], BF16) for i in range(2)])
            qTs = k.sb(es, "qTs", [128, 16, 128], BF16)
            S1 = k.sb(es, "S1", [128, 16, 128], F32)
            S2 = k.sb(es, "S2", [128, 16, 128], F32)
            sv = k.sb(es, "sv", [128, 16, 16], F32)
            si = k.sb(es, "si", [128, 16, 16], U32)
            sif = k.sb(es, "sif", [128, 16, 16], F32)
            cand = k.sb(es, "cand", [128, 8, 256], F32)
            cand2 = k.sb(es, "cand2", [128, 8, 256], F32)
            bv = k.sb(es, "bv", [128, 8, 16], F32)
            pos = k.sb(es, "pos", [128, 8, 16], U32)
            pa = k.sb(es, "pa", [128, 8, 16], U32)
            pb = k.sb(es, "pb", [128, 8, 16], U32)
            paf = k.sb(es, "paf", [128, 8, 16], F32)
            pbf = k.sb(es, "pbf", [128, 8, 16], F32)
            oh = k.sb(es, "oh", [128, 8, 256], F32)
            sel0 = k.sb(es, "sel0", [128, 8, 16], F32)
            sel1 = k.sb(es, "sel1", [128, 8, 16], F32)
            eidx = Rot([k.sb(es, "eidx%d" % i, [128, 128], I32) for i in range(2)])
            gates_ = Rot([k.sb(es, "gate%d" % i, [128, 8, 16], F32) for i in range(2)])
            gsum = k.sb(es, "gsum", [128, 8], F32)
            NG = 16
            guv = Rot([k.sb(es, "guv%d" % i, [128, 2048], BF16) for i in range(NG)])
            actgs = Rot([k.sb(es, "actg%d" % i, [128, 8], F32) for i in range(4)])
            for ab in actgs.bufs:
                ab.cols = [Buf(ab.t[:, j:j + 1], "agc") for j in range(8)]
            wgs8 = Rot([k.sb(es, "wg8_%d" % i, [128, 8], F32) for i in range(4)])
            junks = Rot([k.sb(es, "junkF%d" % i, [128, 1024], BF16) for i in range(6)])
            dgs = Rot([k.sb(es, "dg%d" % i, [128, 128], BF16) for i in range(8)])
            accs = k.sb(es, "accs", [128, 1024], F32)
            xTt = Rot([k.sb(es, "xTt%d" % i, [128, 8, 128], F32) for i in range(2)])
            R1 = Rot(banks[0:4])
            ACC = bank2[2]
            k.skip.remove(dd)
            k.barrier()
            k.release([dd])

            def v3(ap, a, b):
                return ap.rearrange("p (a b) -> p a b", a=a, b=b)

            def stageA(t, st):
                tT = tTs.get()
                k.dma("sp", lambda e: e.dma_start(out=tT[:], in_=TT[:, :, t * 128:(t + 1) * 128].rearrange("c p t -> p c t")), writes=[tT])
                k.op("act", lambda e: e.copy(tTf[:], tT[:]), reads=[tT], writes=[tTf])
                ttok = ttoks.get()
                for hb in range(2):
                    b = R1.get()

                    def tr(e):
                        for q in range(4):
                            ins = e.transpose(b[:, q * 128:(q + 1) * 128], tTf[:, hb * 4 + q, :], ident)
                        return ins
                    k.op("pe", tr, reads=[tTf, cm], writes=[b])
                    k.op("act", lambda e: e.copy(ttok[:, hb * 512:(hb + 1) * 512], b[:]), reads=[b], writes=[ttok])
                yield
                for q4 in range(4):
                    b = R1.get()

                    def mm(e):
                        for j in range(4):
                            hj = q4 * 4 + j
                            for kk in range(8):
                                ins = e.matmul(b[:, j * 128:(j + 1) * 128], wq[:, kk, hj * 128:(hj + 1) * 128], tT[:, kk, :],
                                               start=(kk == 0 and j == 0), stop=(kk == 7), skip_group_check=True)
                        return ins
                    k.op("pe", mm, reads=[wq, tT], writes=[b])
                    k.op("act", lambda e: e.copy(qTs[:, q4 * 4:q4 * 4 + 4, :], v3(b[:], 4, 128)), reads=[b], writes=[qTs])
                for q4 in range(4):
                    b = R1.get()

                    def mm(e):
                        for j in range(4):
                            hj = q4 * 4 + j
                            ins = e.matmul(b[:, j * 128:(j + 1) * 128], qTs[:, hj, :], sk[:, hj, :], start=(j == 0), stop=True, skip_group_check=True)
                        return ins
                    k.op("pe", mm, reads=[qTs, sk], writes=[b])
                    k.op("act", lambda e: e.copy(S1[:, q4 * 4:q4 * 4 + 4, :], v3(b[:], 4, 128)), reads=[b], writes=[S1])
                yield
                for hj in range(16):
                    k.op("dve", lambda e: e.max(out=sv[:, hj, 0:8], in_=S1[:, hj, :]), reads=[S1], writes=[sv])
                    k.op("dve", lambda e: e.max_index(out=si[:, hj, 0:8], in_max=sv[:, hj, 0:8], in_values=S1[:, hj, :]), reads=[S1, sv], writes=[si])
                    k.op("dve", lambda e: e.match_replace(out=S2[:, hj, :], in_to_replace=sv[:, hj, 0:8], in_values=S1[:, hj, :], imm_value=-1e30),
                         reads=[S1, sv], writes=[S2])
                    k.op("dve", lambda e: e.max(out=sv[:, hj, 8:16], in_=S2[:, hj, :]), reads=[S2], writes=[sv])
                    k.op("dve", lambda e: e.max_index(out=si[:, hj, 8:16], in_max=sv[:, hj, 8:16], in_values=S2[:, hj, :]), reads=[S2, sv], writes=[si])
                    if hj % 2 == 1:
                        yield
                k.op("dve", lambda e: e.tensor_copy(sif[:], si[:]), reads=[si], writes=[sif])
                sv4 = sv[:].rearrange("p (h j) a -> p h j a", j=2)
                sif4 = sif[:].rearrange("p (h j) a -> p h j a", j=2)
                k.op("dve", lambda e: e.tensor_tensor(cand[:].rearrange("p h (a b) -> p h a b", a=16), sv4[:, :, 0, :].unsqueeze(3).to_broadcast([128, 8, 16, 16]),
                                                      sv4[:, :, 1, :].unsqueeze(2).to_broadcast([128, 8, 16, 16]), ALU.add), reads=[sv], writes=[cand])
                for h in range(8):
                    k.op("dve", lambda e: e.max(out=bv[:, h, 0:8], in_=cand[:, h, :]), reads=[cand], writes=[bv])
                    k.op("dve", lambda e: e.max_index(out=pos[:, h, 0:8], in_max=bv[:, h, 0:8], in_values=cand[:, h, :]), reads=[cand, bv], writes=[pos])
                    k.op("dve", lambda e: e.match_replace(out=cand2[:, h, :], in_to_replace=bv[:, h, 0:8], in_values=cand[:, h, :], imm_value=-1e30),
                         reads=[cand, bv], writes=[cand2])
                    k.op("dve", lambda e: e.max(out=bv[:, h, 8:16], in_=cand2[:, h, :]), reads=[cand2], writes=[bv])
                    k.op("dve", lambda e: e.max_index(out=pos[:, h, 8:16], in_max=bv[:, h, 8:16], in_values=cand2[:, h, :]), reads=[cand2, bv], writes=[pos])
                    if h % 2 == 1:
                        yield
                k.op("dve", lambda e: e.tensor_scalar(pa[:], pos[:], 4, None, ALU.logical_shift_right), reads=[pos], writes=[pa])
                k.op("dve", lambda e: e.tensor_scalar(pb[:], pos[:], 15, None, ALU.bitwise_and), reads=[pos], writes=[pb])
                k.op("dve", lambda e: e.tensor_copy(paf[:], pa[:]), reads=[pa], writes=[paf])
                k.op("dve", lambda e: e.tensor_copy(pbf[:], pb[:]), reads=[pb], writes=[pbf])
                yield
                oh4 = oh[:].rearrange("p h (r a) -> p h r a", r=16)
                io4 = iota16[:].unsqueeze(1).unsqueeze(1).to_broadcast([128, 8, 16, 16])
                for (pf, jj, sel) in ((paf, 0, sel0), (pbf, 1, sel1)):
                    k.op("dve", lambda e: e.tensor_tensor(oh4, pf[:].unsqueeze(3).to_broadcast([128, 8, 16, 16]), io4, ALU.is_equal), reads=[pf, iota16], writes=[oh])
                    k.op("dve", lambda e: e.tensor_tensor(oh4, oh4, sif4[:, :, jj, :].unsqueeze(2).to_broadcast([128, 8, 16, 16]), ALU.mult), reads=[oh, sif], writes=[oh])
                    k.op("dve", lambda e: e.tensor_reduce(sel[:], oh4, AX.X, ALU.add), reads=[oh], writes=[sel])
                    yield
                yield
                ei = eidx.get()
                gate = gates_.get()
                k.op("dve", lambda e: e.scalar_tensor_tensor(sel0[:], sel0[:], 128.0, sel1[:], ALU.mult, ALU.add), reads=[sel0, sel1], writes=[sel0])
                k.op("dve", lambda e: e.tensor_copy(ei[:], sel0[:].rearrange("p h r -> p (h r)")), reads=[sel0], writes=[ei])
                k.op("dve", lambda e: e.tensor_tensor(gate[:], bv[:], bv[:, :, 0:1].to_broadcast([128, 8, 16]), ALU.subtract), reads=[bv], writes=[gate])
                k.op("act", lambda e: e.activation(gate[:], gate[:], AF.Exp), reads=[gate], writes=[gate])
                k.op("dve", lambda e: e.tensor_reduce(gsum[:], gate[:], AX.X, ALU.add), reads=[gate], writes=[gsum])
                k.op("dve", lambda e: e.reciprocal(gsum[:], gsum[:]), reads=[gsum], writes=[gsum])
                k.op("dve", lambda e: e.tensor_tensor(gate[:], gate[:], gsum[:].unsqueeze(2).to_broadcast([128, 8, 16]), ALU.mult), reads=[gate, gsum], writes=[gate])
                st.update(ttok=ttok, ei=ei, gate=gate)

            def stageUV(t, st, gen):
                w = 1 if t < 2 else 0
                ttok, ei, gate = st['ttok'], st['ei'], st['gate']
                gflat = gate[:].rearrange("p h r -> p (h r)")
                xt_ = xTt.get()
                k.dma("sp", lambda e: e.dma_start(out=xt_[:], in_=XT[:, :, t * 128:(t + 1) * 128].rearrange("c p t -> p c t")), writes=[xt_])

                for g in range(16):
                    gb = []
                    ag = actgs.get()
                    for j in range(8):
                        m = g * 8 + j
                        gu = guv.get()
                        jk = junks.get()
                        k.dma("pool", lambda e: e.indirect_dma_start(out=gu[:], out_offset=None, in_=UV,
                                                                     in_offset=bass.IndirectOffsetOnAxis(ap=ei[:, m:m + 1], axis=0)), reads=[ei], writes=[gu])
                        if j in (2, 6):
                            k.op("dve", lambda e: e.tensor_tensor(jk[:], gu[:, 0:1024], ttok[:], ALU.mult), reads=[gu, ttok], writes=[jk])
                            k.op("act", lambda e: e.activation(jk[:], jk[:], AF.Copy, accum_out=ag.t[:, j:j + 1]), reads=[jk], writes=[jk, ag.cols[j]])
                        else:
                            k.op("dve", lambda e: e.scalar_tensor_tensor(jk[:], gu[:, 0:1024], 1.0, ttok[:], ALU.mult, ALU.mult, accum_out=ag.t[:, j:j + 1]),
                                 reads=[gu, ttok], writes=[jk, ag.cols[j]])
                        gb.append(gu)
                    k.op("act", lambda e: e.activation(ag.t[:], ag.t[:], AF.Gelu), reads=ag.cols, writes=ag.cols)
                    w8 = wgs8.get()
                    for j in range(8):
                        m = g * 8 + j
                        k.op("act", lambda e: e.activation(w8[:, j:j + 1], ag.t[:, j:j + 1], AF.Copy, scale=gflat[:, m:m + 1]), reads=[ag.cols[j], gate], writes=[w8])
                    for j in range(8):
                        m = g * 8 + j
                        gv = gb[j]
                        dg = dgs.get()
                        k.op("act", lambda e: e.activation(dg[:], identb[:], AF.Copy, scale=w8[:, j:j + 1]), reads=[identb, w8], writes=[dg])

                        def mm(e):
                            e.matmul(ACC[:, 0, :], dg[:], gv[:, 1024:1536], start=(m == 0), stop=(m == 127), skip_group_check=True)
                            return e.matmul(ACC[:, 1, :], dg[:], gv[:, 1536:2048], start=(m == 0), stop=(m == 127), skip_group_check=True)
                        k.op("pe", mm, reads=[dg, gv], writes=[ACC])
                    if gen is not None and g >= 1:
                        for _ in range(2):
                            next(gen, None)
                k.op("act", lambda e: e.copy(accs[:], ACC[:].rearrange("p g c -> p (g c)")), reads=[ACC], writes=[accs])
                for hb in range(2):
                    b = R1.get()

                    def tr(e):
                        for q in range(4):
                            cc = hb * 4 + q
                            ins = e.transpose(b[:, q * 128:(q + 1) * 128], accs[:, cc * 128:(cc + 1) * 128], ident)
                        return ins
                    k.op("pe", tr, reads=[accs, cm], writes=[b])
                    for q in range(4):
                        cc = hb * 4 + q
                        k.op("dve", lambda e: e.scalar_tensor_tensor(xt_[:, cc, :], b[:, q * 128:(q + 1) * 128], modcol(5, cc, w), xt_[:, cc, :], ALU.mult, ALU.add),
                             reads=[b, mods, xt_], writes=[xt_])
                k.dma("sp", lambda e: e.dma_start(out=XT[:, :, t * 128:(t + 1) * 128].rearrange("c p t -> p c t"), in_=xt_[:]), reads=[xt_])

            cur = {}
            for _ in stageA(0, cur):
                pass
            for t in range(NT):
                nst = {}
                gen = stageA(t + 1, nst) if t + 1 < NT else None
                stageUV(t, cur, gen)
                if gen is not None:
                    for _ in gen:
                        pass
                cur = nst
            k.barrier()
        lay.close()
        if stop == "F" and l == 0:
            return done()

    with ExitStack() as es:
        gf = k.sb(es, "gf", [128, 8], F32)
        k.dma("sp", lambda e: e.dma_start(out=gf[:], in_=gfin_in), writes=[gf])
        xg = Rot([k.sb(es, "gxg%d" % i, [128, 8, 512], F32) for i in range(2)])
        sq = k.sb(es, "gsq", [128, 8, 512], F32)
        rs = k.sb(es, "grs", [128, 512], F32)
        yT = k.sb(es, "gyT", [128, 8, 512], F32)
        ots = Rot([k.sb(es, "got%d" % i, [128, 1024], F32) for i in range(2)])
        bk = Rot(banks)
        for (t0, n) in groups[1:]:
            x = xg.get()
            k.dma("sp", lambda e: e.dma_start(out=x[:, :, 0:n], in_=XT[:, :, t0:t0 + n].rearrange("c p t -> p c t")), writes=[x])
            k.op("act", lambda e: e.activation(sq[:, :, 0:n], x[:, :, 0:n], AF.Square), reads=[x], writes=[sq])
            b = bk.get()

            def mm(e):
                for c in range(8):
                    ins = e.matmul(b[:, 0:n], C(CM_ONES), sq[:, c, 0:n], start=(c == 0), stop=(c == 7))
                return ins
            k.op("pe", mm, reads=[sq, cm], writes=[b])
            k.op("dve", lambda e: e.tensor_scalar(rs[:, 0:n], b[:, 0:n], 1.0 / D, EPS, ALU.mult, ALU.add), reads=[b], writes=[rs])
            k.op("act", lambda e: e.activation(rs[:, 0:n], rs[:, 0:n], AF.Sqrt), reads=[rs], writes=[rs])
            k.op("dve", lambda e: e.reciprocal(rs[:, 0:n], rs[:, 0:n]), reads=[rs], writes=[rs])
            for c in range(8):
                k.op("dve", lambda e: e.scalar_tensor_tensor(yT[:, c, 0:n], x[:, c, 0:n], gf[:, c:c + 1], rs[:, 0:n], ALU.mult, ALU.mult),
                     reads=[x, gf, rs], writes=[yT])
            for q in range(n // 128):
                ot = ots.get()
                for hb in range(2):
                    b = bk.get()

                    def tr(e):
                        for j in range(4):
                            ins = e.transpose(b[:, j * 128:(j + 1) * 128], yT[:, hb * 4 + j, q * 128:(q + 1) * 128], ident)
                        return ins
                    k.op("pe", tr, reads=[yT, cm], writes=[b])
                    k.op("act" if hb else "dve", lambda e: (e.copy if hb else e.tensor_copy)(ot[:, hb * 512:(hb + 1) * 512], b[:]), reads=[b], writes=[ot])
                r0 = t0 - 256 + q * 128
                k.dma("sp", lambda e: e.dma_start(out=out[r0:r0 + 128, :], in_=ot[:]), reads=[ot])
        k.barrier()
    return done()


_CACHE = {}


def kernel(**inputs):
    L, nxt = 4, 32
    B = 8
    inp = {kk: np.asarray(v) for kk, v in inputs.items()}
    shared = host_prep(inp, L, nxt)
    if "nc" not in _CACHE:
        _CACHE["nc"] = build(L, nxt)
    nc = _CACHE["nc"]
    in_maps = []
    for b in range(B):
        m = dict(shared)
        m["x"] = np.ascontiguousarray(inp["x"][b], np.float32)
        m["ctx"] = np.ascontiguousarray(inp["ctx"][b], np.float32)
        cv = np.stack([np.asarray(inp["c"][b], np.float32).reshape(8, 128).T,
                       np.asarray(inp["c_ctx"], np.float32).reshape(8, 128).T], axis=-1)
        m["cvec"] = np.ascontiguousarray(cv)
        in_maps.append(m)
    res = run_bass_kernel_spmd(nc, in_maps, core_ids=list(range(B)))
    return np.stack([np.asarray(r["out"], np.float32) for r in res.results], axis=0)
```

```python
import math
import numpy as np
from contextlib import ExitStack
import concourse.bass as bass
import concourse.mybir as mybir
from concourse.bass_utils import run_bass_kernel_spmd

F32 = mybir.dt.float32
BF16 = mybir.dt.bfloat16
I32 = mybir.dt.int32
U32 = mybir.dt.uint32
AF = mybir.ActivationFunctionType
ALU = mybir.AluOpType
AX = mybir.AxisListType

D = 1024
HD = 64
EPS = 1e-6
IN_PARTS = (("a_q", 512), ("a_k", 128), ("a_v", 128), ("b_q", 512), ("b_k", 128), ("b_v", 128),
            ("c_q", 512), ("c_k", 512), ("c_v", 512), ("d_z", 1024), ("d_xbc", 1536), ("d_dt", 32),
            ("g_a", 1024), ("g_b", 1024), ("g_c", 1024), ("g_d", 1024))
OFF = {}
_o = 0
for _n, _w in IN_PARTS:
    OFF[_n] = _o
    _o += _w
IN_W = _o
ROTP = np.array([d + 16 if (d // 16) % 2 == 0 else d - 16 for d in range(64)])
SIGN = np.array([-1.0 if (d // 16) % 2 == 0 else 1.0 for d in range(64)], np.float32)
NFMG = 21
TMW = 1824
VW = 776
QK_AQ, QK_AK, QK_BQ, QK_BK, QK_CQ, QK_CK = 0, 4, 6, 10, 12, 16
NPP = 120
NFP = 1504


def fm_colmap():
    cols = []

    def pair(base, h0, h1):
        p = np.concatenate([base + h0 * 64 + np.arange(64), base + h1 * 64 + np.arange(64)])
        r = np.concatenate([base + h0 * 64 + ROTP, base + h1 * 64 + ROTP])
        cols.append(p)
        cols.append(r)

    for i in range(4):
        pair(OFF["a_q"], 2 * i, 2 * i + 1)
    for j in range(2):
        pair(OFF["a_k"], j, j)
    for i in range(4):
        pair(OFF["b_q"], 2 * i, 2 * i + 1)
    for j in range(2):
        pair(OFF["b_k"], j, j)
    for i in range(4):
        pair(OFF["c_q"], 2 * i, 2 * i + 1)
    for i in range(4):
        pair(OFF["c_k"], 2 * i, 2 * i + 1)
    for c in range(12):
        cols.append(OFF["d_xbc"] + c * 128 + np.arange(128))
    for c in range(32):
        cols.append(OFF["g_a"] + c * 128 + np.arange(128))
    return np.concatenate(cols)


def tm_colmap():
    return np.concatenate([OFF["a_v"] + np.arange(128), OFF["b_v"] + np.arange(128),
                           OFF["c_v"] + np.arange(512), OFF["d_z"] + np.arange(1024),
                           OFF["d_dt"] + np.arange(32)])


class Buf:
    __slots__ = ("t", "lastw", "readers", "dsem", "dcnt", "name", "cols")

    def __init__(self, t, name=""):
        self.t = t
        self.lastw = None
        self.readers = []
        self.dsem = None
        self.dcnt = 0
        self.name = name

    def __getitem__(self, idx):
        return self.t[idx]


class MK:
    ENG = ("pe", "act", "dve", "pool", "sp")

    def __init__(self, nc):
        self.nc = nc
        self.es = ExitStack()
        self.e = {"pe": nc.tensor, "act": nc.scalar, "dve": nc.vector, "pool": nc.gpsimd, "sp": nc.sync}
        self.sem = {k: self.es.enter_context(nc.semaphore("c_" + k)) for k in self.ENG}
        self.cnt = {k: 0 for k in self.ENG}
        self.waited = {k: {} for k in self.ENG}
        self.dsems = []
        self.free_dsems = []
        self.dma_bufs = []
        self.nwaits = 0
        self.ninst = 0
        self.skip = []

    def sb(self, es, name, shape, dt):
        self.nsb = getattr(self, "nsb", 0) + 1
        name = "s%d_%s" % (self.nsb, name)
        b = Buf(es.enter_context(self.nc.sbuf_tensor(name, list(shape), dt)), name)
        es.callback(self.release, [b])
        return b

    def _dsem(self, b):
        if b.dsem is None:
            if self.free_dsems:
                b.dsem, b.dcnt = self.free_dsems.pop()
            else:
                b.dsem = self.es.enter_context(self.nc.semaphore("d%d" % len(self.dsems)))
                self.dsems.append(b.dsem)
                b.dcnt = 0
            self.dma_bufs.append(b)
        return b.dsem

    def release(self, bufs):
        for b in bufs:
            if b.dsem is not None:
                self.free_dsems.append((b.dsem, b.dcnt))
                b.dsem = None
                self.dma_bufs.remove(b)

    def _wait(self, eng, tok):
        sem, val = tok
        if eng == "pe" and sem is self.sem["pe"]:
            return
        w = self.waited[eng]
        key = id(sem)
        if w.get(key, 0) >= val:
            return
        w[key] = val
        self.e[eng].wait_ge(sem, val)
        self.nwaits += 1

    def _deps(self, eng, reads, writes):
        for b in reads:
            if b.lastw is not None:
                self._wait(eng, b.lastw)
        for b in writes:
            if b.lastw is not None:
                self._wait(eng, b.lastw)
            for t in b.readers:
                self._wait(eng, t)

    def _commit(self, tok, reads, writes):
        for b in reads:
            b.readers.append(tok)
            if len(b.readers) > 48:
                best = {}
                for s, v in b.readers:
                    if best.get(id(s), (None, -1))[1] < v:
                        best[id(s)] = (s, v)
                b.readers = list(best.values())
        for b in writes:
            b.lastw = tok
            b.readers = []

    def op(self, eng, fn, reads=(), writes=()):
        self._deps(eng, reads, writes)
        ins = fn(self.e[eng])
        self.cnt[eng] += 1
        ins.then_inc(self.sem[eng], 1)
        tok = (self.sem[eng], self.cnt[eng])
        self._commit(tok, reads, writes)
        self.ninst += 1
        return tok

    def dma(self, eng, fn, reads=(), writes=()):
        self._deps(eng, reads, writes)
        bufs = list(writes) + list(reads)
        b0 = bufs[0]
        sem = self._dsem(b0)
        ins = fn(self.e[eng])
        b0.dcnt += 16
        ins.then_inc(sem, 16)
        tok = (sem, b0.dcnt)
        self._commit(tok, reads, writes)
        self.ninst += 1
        return tok

    def barrier(self):
        toks = [(self.sem[k], self.cnt[k]) for k in self.ENG if self.cnt[k] > 0]
        for b in self.dma_bufs:
            if b.dsem is not None and b.dcnt > 0 and b not in self.skip:
                toks.append((b.dsem, b.dcnt))
        for k in self.ENG:
            for t in toks:
                self._wait(k, t)


class Rot:
    def __init__(self, bufs):
        self.bufs = bufs
        self.i = 0

    def get(self):
        b = self.bufs[self.i % len(self.bufs)]
        self.i += 1
        return b


def host_consts(nxt):
    ntok = 128 * (2 + nxt)
    k = np.arange(128)
    U = (k[:, None] <= k[None, :]).astype(np.float32)
    Lst = (k[:, None] > k[None, :]).astype(np.float32)
    Lo = (k[:, None] >= k[None, :]).astype(np.float32)
    Ust = (k[:, None] < k[None, :]).astype(np.float32)
    ident = np.eye(128, dtype=np.float32)
    ones = np.ones((128, 128), np.float32)
    blk = np.zeros((128, 128), np.float32)
    blk[:64, :64] = 1
    blk[64:, 64:] = 1
    cm = np.concatenate([ident, U, Lst, Lo, Ust, ones, blk], axis=1)
    s = np.arange(128 * nxt)
    row = (s // 64).astype(np.float32)
    col = (s % 64).astype(np.float32)
    nq = 16
    inv = (10000.0 ** (-np.arange(nq, dtype=np.float32) / nq)).astype(np.float32)
    ar = row[:, None] * inv
    ac = col[:, None] * inv
    ang = np.concatenate([ar, ar, ac, ac], axis=-1)
    cos = np.cos(ang).astype(np.float32)
    sin = np.sin(ang).astype(np.float32)
    cosT = np.ones((128, ntok), np.float32)
    sinT = np.zeros((128, ntok), np.float32)
    cosT[:, 256:] = np.concatenate([cos.T, cos.T], axis=0)
    sinT[:, 256:] = np.concatenate([(sin * SIGN[None, :]).T, (sin * SIGN[None, :]).T], axis=0)
    return cm, cosT, sinT


CM_ID, CM_U, CM_LST, CM_LO, CM_UST, CM_ONES, CM_BLK = [i * 128 for i in range(7)]


def host_prep(inp, L, nxt):
    f = np.float32
    fmc = fm_colmap()
    tmc = tm_colmap()
    w_in = np.asarray(inp["w_in"], f)
    wfm = np.empty((L, NFMG, 128, 8, 512), f)
    wtm = np.empty((L, 128, 8, TMW), f)
    for l in range(L):
        g = w_in[l][:, fmc].reshape(8, 128, NFMG, 512)
        wfm[l] = g.transpose(2, 1, 0, 3)
        wtm[l] = w_in[l][:, tmc].reshape(8, 128, TMW).transpose(1, 0, 2)
    pp = np.zeros((L, 128, NPP), f)
    fp = np.zeros((L, 1, NFP), f)
    p64 = np.arange(128) % 64
    for l in range(L):
        pp[l, :, 0] = inp["b_qnorm"][l][p64]
        pp[l, :, 1] = inp["b_qnorm"][l][ROTP[p64]]
        pp[l, :, 2] = inp["b_knorm"][l][p64]
        pp[l, :, 3] = inp["b_knorm"][l][ROTP[p64]]
        cw = np.asarray(inp["m_conv_w"][l], f)
        for j in range(3):
            pp[l, :, 4 + 12 * j: 16 + 12 * j] = cw[j].reshape(12, 128).T
        pp[l, :, 40:52] = np.asarray(inp["m_conv_b"][l], f).reshape(12, 128).T
        pp[l, :, 52:60] = np.asarray(inp["g_norm1"][l], f).reshape(8, 128).T
        pp[l, :, 60:68] = np.asarray(inp["g_norm2"][l], f).reshape(8, 128).T
        pp[l, :, 68:116] = np.asarray(inp["b_ada"][l], f).reshape(48, 128).T
        fpl = fp[l, 0]
        fpl[0:8] = inp["a_sink"][l]
        fpl[8:72] = inp["c_lam_q1"][l]
        fpl[72:136] = inp["c_lam_k1"][l]
        fpl[136:200] = inp["c_lam_q2"][l]
        fpl[200:264] = inp["c_lam_k2"][l]
        fpl[264:392] = inp["c_subln"][l]
        fpl[392:424] = np.asarray(inp["m_dt_bias"][l], f).reshape(32)
        fpl[424:456] = np.asarray(inp["m_a_log"][l], f).reshape(32)
        fpl[456:472] = inp["m_d"][l]
        fpl[472:1496] = inp["m_norm"][l]
    gfin = np.asarray(inp["g_final"], f).reshape(8, 128).T.copy()
    skt = np.ascontiguousarray(np.asarray(inp["p_subkeys"], f).reshape(L, 16, 128, 128).transpose(0, 3, 1, 2))
    shared = {
        "wfm": wfm, "wtm": wtm, "pp": pp, "fp": fp, "gfin": gfin, "skt": skt,
        "w_ada": np.ascontiguousarray(inp["w_ada"], f), "w_br": np.ascontiguousarray(inp["w_br"], f),
        "w_out": np.ascontiguousarray(inp["w_out"], f), "p_wq": np.ascontiguousarray(inp["p_wq"], f),
        "p_u": np.ascontiguousarray(inp["p_u"], f), "p_v": np.ascontiguousarray(inp["p_v"], f),
    }
    cm, cosT, sinT = host_consts(nxt)
    shared.update({"cm": cm, "cosT": cosT, "sinT": sinT})
    return shared


def build(L, nxt, debug=False, stop=None, lam_inits=None):
    NT = 2 + nxt
    ntok = 128 * NT
    groups = [(0, 256)] + [(256 + 512 * i, 512) for i in range(nxt // 4)]
    nc = bass.Bass("TRN2", target_bir_lowering=False)
    k = MK(nc)

    def din(name, shape, dt=F32):
        return nc.dram_tensor(name, list(shape), dt, kind="ExternalInput").ap()

    def scr(name, shape, dt):
        return nc.dram_tensor(name, list(shape), dt, kind="ExternalOutput" if debug else "Internal").ap()

    x_in = din("x", [128 * nxt, D])
    ctx_in = din("ctx", [256, D])
    cvec = din("cvec", [128, 8, 2])
    wfm = din("wfm", [L, NFMG, 128, 8, 512])
    wtm = din("wtm", [L, 128, 8, TMW])
    pp_in = din("pp", [L, 128, NPP])
    fp_in = din("fp", [L, 1, NFP])
    gfin_in = din("gfin", [128, 8])
    skt_in = din("skt", [L, 128, 16, 128])
    w_ada = din("w_ada", [L, D, 6 * D])
    w_br = din("w_br", [L, 2560, D])
    w_out = din("w_out", [L, D, D])
    p_wq = din("p_wq", [L, D, 2048])
    p_u = din("p_u", [L, 16384, D])
    p_v = din("p_v", [L, 16384, D])
    cm_in = din("cm", [128, 896])
    cos_in = din("cosT", [128, ntok])
    sin_in = din("sinT", [128, ntok])
    out = nc.dram_tensor("out", [128 * nxt, D], F32, kind="ExternalOutput").ap()
    XT = scr("XT", [8, 128, ntok], F32)
    QK = scr("QK", [20, 128, ntok], BF16)
    VAUG = scr("VAUG", [NT, 128, VW], BF16)
    XTOK = scr("XTOK", [NT, 128, 1024], BF16)
    BTOK = scr("BTOK", [NT, 128, 256], BF16)
    BT = scr("BT", [2, 128, ntok], BF16)
    CT = scr("CT", [2, 128, ntok], BF16)
    ZS = scr("ZS", [NT, 128, 1024], BF16)
    DT = scr("DT", [NT, 128, 32], F32)
    GT = scr("GT", [32, 128, ntok], BF16)
    OT = scr("OT", [20, 128, ntok], BF16)
    YF = scr("YF", [NT, 128, 1024], F32)
    TT = scr("TT", [8, 128, ntok], BF16)
    UV = scr("UV", [16384, 2 * D], BF16)
    MODS = scr("MODS", [128, 48, 2], F32) if debug else None

    top = ExitStack()
    cm = k.sb(top, "cm", [128, 896], F32)
    ps_all = top.enter_context(nc.psum_tensor("ps", [128, 8, 512], F32))
    banks = [Buf(ps_all[:, i, :], "bank%d" % i) for i in range(8)]
    bank2 = [Buf(ps_all[:, 2 * i:2 * i + 2, :], "bank2_%d" % i) for i in range(4)]
    k.dma("sp", lambda e: e.dma_start(out=cm[:], in_=cm_in), writes=[cm])

    def C(off, n=128):
        return cm[:, off:off + n]

    ident = C(CM_ID)

    def done():
        k.barrier()
        print("MK: ninst", k.ninst, "nwaits", k.nwaits, "dsems", len(k.dsems), flush=True)
        top.close()
        k.es.close()
        return nc

    with ExitStack() as es:
        xin = Rot([k.sb(es, "xin%d" % i, [128, D], F32) for i in range(2)])
        xo = Rot([k.sb(es, "xo%d" % i, [128, 8, 128], F32) for i in range(2)])
        bk = Rot(banks)
        for t in range(NT):
            src = ctx_in[t * 128:(t + 1) * 128, :] if t < 2 else x_in[(t - 2) * 128:(t - 1) * 128, :]
            xi = xin.get()
            k.dma("sp", lambda e: e.dma_start(out=xi[:], in_=src), writes=[xi])
            xob = xo.get()
            for half in range(2):
                b = bk.get()

                def tr(e):
                    for c in range(4):
                        ins = e.transpose(b[:, c * 128:(c + 1) * 128], xi[:, (half * 4 + c) * 128:(half * 4 + c + 1) * 128], ident)
                    return ins
                k.op("pe", tr, reads=[xi, cm], writes=[b])
                k.op("act" if half else "dve", lambda e: (e.copy if half else e.tensor_copy)(
                    xob[:, half * 4:half * 4 + 4, :], b[:].rearrange("p (c t) -> p c t", c=4)), reads=[b], writes=[xob])
            k.dma("sp", lambda e: e.dma_start(out=XT[:, :, t * 128:(t + 1) * 128].rearrange("c p t -> p c t"), in_=xob[:]), reads=[xob])
        k.barrier()
    if stop == "I":
        return done()

    for l in range(L):
        lam_init = 0.8 - 0.6 * math.exp(-0.3 * l)
        lay = ExitStack()
        pp = k.sb(lay, "pp", [128, NPP], F32)
        fpb = k.sb(lay, "fpb", [128, NFP], F32)
        k.dma("sp", lambda e: e.dma_start(out=pp[:], in_=pp_in[l]), writes=[pp])
        k.dma("sp", lambda e: e.dma_start(out=fpb[:], in_=fp_in[l].partition_broadcast(128)), writes=[fpb])
        mods = k.sb(lay, "mods", [128, 48, 2], F32)
        gs1 = k.sb(lay, "gs1", [128, 8, 2], F32)
        gs2 = k.sb(lay, "gs2", [128, 8, 2], F32)

        with ExitStack() as es:
            sc = k.sb(es, "sc", [128, 8, 2], F32)
            k.dma("sp", lambda e: e.dma_start(out=sc[:], in_=cvec), writes=[sc])
            k.op("act", lambda e: e.activation(sc[:], sc[:], AF.Silu), reads=[sc], writes=[sc])
            was = Rot([k.sb(es, "wa%d" % i, [128, 8, 768], F32) for i in range(2)])
            mb = banks[0]
            for cg in range(8):
                wa = was.get()
                k.dma("sp" if cg % 2 else "act", lambda e: e.dma_start(
                    out=wa[:], in_=w_ada[l].rearrange("(k p) c -> p k c", p=128)[:, :, cg * 768:(cg + 1) * 768]), writes=[wa])

                def mm(e):
                    for j in range(6):
                        ch = cg * 6 + j
                        for kk in range(8):
                            ins = e.matmul(mb[:, ch * 2:ch * 2 + 2], wa[:, kk, j * 128:(j + 1) * 128], sc[:, kk, :],
                                           start=(kk == 0), stop=(kk == 7), skip_group_check=True)
                    return ins
                k.op("pe", mm, reads=[wa, sc], writes=[mb])
            k.op("dve", lambda e: e.tensor_tensor(mods[:], mb[:, 0:96].rearrange("p (c w) -> p c w", w=2),
                                                  pp[:, 68:116].unsqueeze(2).to_broadcast([128, 48, 2]), ALU.add),
                 reads=[mb, pp], writes=[mods])
            for (gs, part, gcol) in ((gs1, 1, 52), (gs2, 4, 60)):
                k.op("dve", lambda e: e.tensor_scalar(gs[:], mods[:, part * 8:part * 8 + 8, :], 1.0, None, ALU.add),
                     reads=[mods], writes=[gs])
                k.op("dve", lambda e: e.tensor_tensor(gs[:], gs[:], pp[:, gcol:gcol + 8].unsqueeze(2).to_broadcast([128, 8, 2]), ALU.mult),
                     reads=[gs, pp], writes=[gs])
            if debug:
                k.dma("sp", lambda e: e.dma_start(out=MODS, in_=mods[:]), reads=[mods])
            k.barrier()
        if stop == "M":
            lay.close()
            return done()

        def modcol(part, c, w):
            return mods[:, part * 8 + c, w:w + 1]

        with ExitStack() as es:
            hT = k.sb(es, "hT", [128, 8, ntok], BF16)
            cosT = k.sb(es, "cosT", [128, ntok], F32)
            sinT = k.sb(es, "sinT", [128, ntok], F32)
            k.dma("sp", lambda e: e.dma_start(out=cosT[:], in_=cos_in), writes=[cosT])
            k.dma("sp", lambda e: e.dma_start(out=sinT[:], in_=sin_in), writes=[sinT])
            bk = Rot(banks)
            with ExitStack() as es1:
                xg = Rot([k.sb(es1, "xg%d" % i, [128, 8, 512], F32) for i in range(2)])
                sq = k.sb(es1, "sq", [128, 8, 512], F32)
                rs = k.sb(es1, "rs", [128, 512], F32)
                tmp = Rot([k.sb(es1, "a1tmp%d" % i, [128, 512], F32) for i in range(2)])
                for (t0, n) in groups:
                    w = 1 if t0 == 0 else 0
                    x = xg.get()
                    k.dma("sp", lambda e: e.dma_start(out=x[:, :, 0:n], in_=XT[:, :, t0:t0 + n].rearrange("c p t -> p c t")), writes=[x])
                    k.op("act", lambda e: e.activation(sq[:, :, 0:n], x[:, :, 0:n], AF.Square), reads=[x], writes=[sq])
                    b = bk.get()

                    def mm(e):
                        for c in range(8):
                            ins = e.matmul(b[:, 0:n], C(CM_ONES), sq[:, c, 0:n], start=(c == 0), stop=(c == 7))
                        return ins
                    k.op("pe", mm, reads=[sq, cm], writes=[b])
                    k.op("dve", lambda e: e.tensor_scalar(rs[:, 0:n], b[:, 0:n], 1.0 / D, EPS, ALU.mult, ALU.add), reads=[b], writes=[rs])
                    k.op("act", lambda e: e.activation(rs[:, 0:n], rs[:, 0:n], AF.Sqrt), reads=[rs], writes=[rs])
                    k.op("dve", lambda e: e.reciprocal(rs[:, 0:n], rs[:, 0:n]), reads=[rs], writes=[rs])
                    for c in range(8):
                        tb = tmp.get()
                        k.op("dve", lambda e: e.scalar_tensor_tensor(tb[:, 0:n], x[:, c, 0:n], gs1[:, c, w:w + 1], rs[:, 0:n], ALU.mult, ALU.mult),
                             reads=[x, gs1, rs], writes=[tb])
                        k.op("act", lambda e: e.activation(hT[:, c, t0:t0 + n], tb[:, 0:n], AF.Identity, bias=modcol(0, c, w), scale=1.0),
                             reads=[tb, mods], writes=[hT])
                k.barrier()
            if stop == "A1":
                dbg = scr("HT", [128, 8, ntok], BF16)
                k.dma("sp", lambda e: e.dma_start(out=dbg, in_=hT[:]), reads=[hT])
                es.close(); lay.close()
                return done()
            with ExitStack() as es2:
                wgs = Rot([k.sb(es2, "wg%d" % i, [128, 8, 512], BF16) for i in range(2)])
                t1s = Rot([k.sb(es2, "t1_%d" % i, [128, 512], F32) for i in range(2)])
                t2s = Rot([k.sb(es2, "t2_%d" % i, [128, 512], F32) for i in range(2)])
                obs = Rot([k.sb(es2, "ob%d" % i, [128, 512], BF16) for i in range(3)])
                sqb = k.sb(es2, "sqb", [128, 512], F32)
                rsb = k.sb(es2, "rsb", [128, 512], F32)
                xrow = k.sb(es2, "xrow", [128, ntok + 4], F32)
                crow = k.sb(es2, "crow", [128, ntok], F32)
                trb = Rot([k.sb(es2, "trb%d" % i, [128, 4, 128], BF16) for i in range(2)])
                k.op("pool", lambda e: e.memset(xrow[:], 0.0), writes=[xrow])
                nx = 128 * nxt
                XOFF = 259

                def rowcol(t0):
                    return 1 + t0 if t0 < 256 else XOFF + (t0 - 256)

                def mm_chunk(wg, j, t0, n):
                    b = bk.get()

                    def mm(e):
                        for kk in range(8):
                            ins = e.matmul(b[:, 0:n], wg[:, kk, j * 128:(j + 1) * 128], hT[:, kk, t0:t0 + n],
                                           start=(kk == 0), stop=(kk == 7))
                        return ins
                    k.op("pe", mm, reads=[wg, hT], writes=[b])
                    return b

                for g in range(NFMG):
                    wg = wgs.get()
                    k.dma("pool", lambda e: e.dma_start(out=wg[:], in_=wfm[l, g]), writes=[wg])
                    if g < 10:
                        isB = g in (3, 4, 5)
                        for pi in range(2):
                            if g < 2:
                                qk = QK_AQ + g * 2 + pi
                            elif g == 2:
                                qk = QK_AK + pi
                            elif g < 5:
                                qk = QK_BQ + (g - 3) * 2 + pi
                            elif g == 5:
                                qk = QK_BK + pi
                            elif g < 8:
                                qk = QK_CQ + (g - 6) * 2 + pi
                            else:
                                qk = QK_CK + (g - 8) * 2 + pi
                            gcol = 0 if g in (3, 4) else 2
                            for (t0, n) in groups:
                                bp = mm_chunk(wg, 2 * pi, t0, n)
                                br = mm_chunk(wg, 2 * pi + 1, t0, n)
                                t1 = t1s.get()
                                t2 = t2s.get()
                                ob = obs.get()
                                if isB:
                                    k.op("act", lambda e: e.activation(sqb[:, 0:n], bp[:, 0:n], AF.Square), reads=[bp], writes=[sqb])
                                    bs = bk.get()
                                    k.op("pe", lambda e: e.matmul(bs[:, 0:n], C(CM_BLK), sqb[:, 0:n], start=True, stop=True),
                                         reads=[sqb, cm], writes=[bs])
                                    k.op("dve", lambda e: e.tensor_scalar(rsb[:, 0:n], bs[:, 0:n], 1.0 / HD, EPS, ALU.mult, ALU.add),
                                         reads=[bs], writes=[rsb])
                                    k.op("act", lambda e: e.activation(rsb[:, 0:n], rsb[:, 0:n], AF.Sqrt), reads=[rsb], writes=[rsb])
                                    k.op("dve", lambda e: e.reciprocal(rsb[:, 0:n], rsb[:, 0:n]), reads=[rsb], writes=[rsb])
                                    k.op("dve", lambda e: e.scalar_tensor_tensor(t1[:, 0:n], bp[:, 0:n], pp[:, gcol:gcol + 1], rsb[:, 0:n], ALU.mult, ALU.mult),
                                         reads=[bp, pp, rsb], writes=[t1])
                                    k.op("dve", lambda e: e.scalar_tensor_tensor(t2[:, 0:n], br[:, 0:n], pp[:, gcol + 1:gcol + 2], rsb[:, 0:n], ALU.mult, ALU.mult),
                                         reads=[br, pp, rsb], writes=[t2])
                                    k.op("pool", lambda e: e.tensor_tensor(t1[:, 0:n], t1[:, 0:n], cosT[:, t0:t0 + n], ALU.mult), reads=[t1, cosT], writes=[t1])
                                    k.op("pool", lambda e: e.tensor_tensor(t2[:, 0:n], t2[:, 0:n], sinT[:, t0:t0 + n], ALU.mult), reads=[t2, sinT], writes=[t2])
                                else:
                                    k.op("dve", lambda e: e.tensor_tensor(t1[:, 0:n], bp[:, 0:n], cosT[:, t0:t0 + n], ALU.mult), reads=[bp, cosT], writes=[t1])
                                    k.op("dve", lambda e: e.tensor_tensor(t2[:, 0:n], br[:, 0:n], sinT[:, t0:t0 + n], ALU.mult), reads=[br, sinT], writes=[t2])
                                k.op("pool", lambda e: e.tensor_tensor(ob[:, 0:n], t1[:, 0:n], t2[:, 0:n], ALU.add), reads=[t1, t2], writes=[ob])
                                k.dma("sp", lambda e: e.dma_start(out=QK[qk, :, t0:t0 + n], in_=ob[:, 0:n]), reads=[ob])
                    elif g < 13:
                        for j in range(4):
                            c = (g - 10) * 4 + j
                            for (t0, n) in groups:
                                b = mm_chunk(wg, j, t0, n)
                                rc = rowcol(t0)
                                k.op("act", lambda e: e.copy(xrow[:, rc:rc + n], b[:, 0:n]), reads=[b], writes=[xrow])
                            for (s0, sn, o0) in ((1, 256, 0), (XOFF, nx, 256)):
                                k.op("act", lambda e: e.activation(crow[:, o0:o0 + sn], xrow[:, s0 - 1:s0 - 1 + sn], AF.Identity,
                                                                   bias=pp[:, 40 + c:41 + c], scale=pp[:, 4 + c:5 + c]),
                                     reads=[xrow, pp], writes=[crow])
                                k.op("dve", lambda e: e.scalar_tensor_tensor(crow[:, o0:o0 + sn], xrow[:, s0:s0 + sn], pp[:, 16 + c:17 + c],
                                                                             crow[:, o0:o0 + sn], ALU.mult, ALU.add), reads=[xrow, pp, crow], writes=[crow])
                                k.op("dve", lambda e: e.scalar_tensor_tensor(crow[:, o0:o0 + sn], xrow[:, s0 + 1:s0 + 1 + sn], pp[:, 28 + c:29 + c],
                                                                             crow[:, o0:o0 + sn], ALU.mult, ALU.add), reads=[xrow, pp, crow], writes=[crow])
                            k.op("act", lambda e: e.activation(crow[:], crow[:], AF.Silu), reads=[crow], writes=[crow])
                            if c >= 8:
                                dst = BT if c < 10 else CT
                                gi = (c - 8) % 2
                                for (t0, n) in groups:
                                    ob = obs.get()
                                    k.op("pool", lambda e: e.tensor_copy(ob[:, 0:n], crow[:, t0:t0 + n]), reads=[crow], writes=[ob])
                                    k.dma("sp", lambda e: e.dma_start(out=dst[gi, :, t0:t0 + n], in_=ob[:, 0:n]), reads=[ob])
                            if c < 10:
                                for tb0 in range(0, NT, 4):
                                    nt4 = min(4, NT - tb0)
                                    b = bk.get()

                                    def tr(e):
                                        for q in range(nt4):
                                            ins = e.transpose(b[:, q * 128:(q + 1) * 128], crow[:, (tb0 + q) * 128:(tb0 + q + 1) * 128], ident)
                                        return ins
                                    k.op("pe", tr, reads=[crow, cm], writes=[b])
                                    tbf = trb.get()
                                    k.op("dve", lambda e: e.tensor_copy(tbf[:, 0:nt4, :], b[:, 0:nt4 * 128].rearrange("p (q c) -> p q c", q=nt4)),
                                         reads=[b], writes=[tbf])
                                    if c < 8:
                                        dd = XTOK[tb0:tb0 + nt4, :, c * 128:(c + 1) * 128]
                                    else:
                                        dd = BTOK[tb0:tb0 + nt4, :, (c - 8) * 128:(c - 7) * 128]
                                    k.dma("sp", lambda e: e.dma_start(out=dd.rearrange("t p c -> p t c"), in_=tbf[:, 0:nt4, :]), reads=[tbf])
                    else:
                        for j in range(4):
                            gc = (g - 13) * 4 + j
                            for (t0, n) in groups:
                                b = mm_chunk(wg, j, t0, n)
                                ob = obs.get()
                                k.op("act", lambda e: e.activation(ob[:, 0:n], b[:, 0:n], AF.Sigmoid), reads=[b], writes=[ob])
                                k.dma("sp", lambda e: e.dma_start(out=GT[gc, :, t0:t0 + n], in_=ob[:, 0:n]), reads=[ob])
                k.barrier()
            with ExitStack() as es3:
                wt = k.sb(es3, "wt", [128, 8, TMW], BF16)
                k.dma("pool", lambda e: e.dma_start(out=wt[:], in_=wtm[l]), writes=[wt])
                vas = Rot([k.sb(es3, "va%d" % i, [128, VW], BF16) for i in range(2)])
                zss = Rot([k.sb(es3, "zs%d" % i, [128, 1024], BF16) for i in range(2)])
                dts = Rot([k.sb(es3, "dt%d" % i, [128, 32], F32) for i in range(2)])
                for va in vas.bufs:
                    k.op("pool", lambda e: e.memset(va[:], 1.0), writes=[va])
                blocks = ((0, 256), (256, 512), (768, 512), (1280, 512), (1792, 32))
                for t in range(NT):
                    bs = []
                    for (c0, cn) in blocks:
                        b = bk.get()

                        def mm(e):
                            for kk in range(8):
                                ins = e.matmul(b[:, 0:cn], hT[:, kk, t * 128:(t + 1) * 128], wt[:, kk, c0:c0 + cn],
                                               start=(kk == 0), stop=(kk == 7))
                            return ins
                        k.op("pe", mm, reads=[wt, hT], writes=[b])
                        bs.append(b)
                    va = vas.get()
                    zs = zss.get()
                    dtb = dts.get()
                    k.op("dve", lambda e: e.tensor_copy(va[:, 0:260].rearrange("p (h w) -> p h w", w=65)[:, :, 0:64],
                                                        bs[0][:, 0:256].rearrange("p (h w) -> p h w", w=64)), reads=[bs[0]], writes=[va])
                    k.op("dve", lambda e: e.tensor_copy(va[:, 260:776].rearrange("p (h w) -> p h w", w=129)[:, :, 0:128],
                                                        bs[1][:, 0:512].rearrange("p (h w) -> p h w", w=128)), reads=[bs[1]], writes=[va])
                    k.op("act", lambda e: e.activation(zs[:, 0:512], bs[2][:, 0:512], AF.Silu), reads=[bs[2]], writes=[zs])
                    k.op("act", lambda e: e.activation(zs[:, 512:1024], bs[3][:, 0:512], AF.Silu), reads=[bs[3]], writes=[zs])
                    k.op("dve", lambda e: e.tensor_tensor(dtb[:], bs[4][:, 0:32], fpb[:, 392:424], ALU.add), reads=[bs[4], fpb], writes=[dtb])
                    k.op("act", lambda e: e.activation(dtb[:], dtb[:], AF.Exp), reads=[dtb], writes=[dtb])
                    k.op("act", lambda e: e.activation(dtb[:], dtb[:], AF.Ln, bias=1.0, scale=1.0), reads=[dtb], writes=[dtb])
                    k.dma("sp", lambda e: e.dma_start(out=VAUG[t], in_=va[:]), reads=[va])
                    k.dma("sp", lambda e: e.dma_start(out=ZS[t], in_=zs[:]), reads=[zs])
                    k.dma("sp", lambda e: e.dma_start(out=DT[t], in_=dtb[:]), reads=[dtb])
                k.barrier()
        if stop == "A":
            lay.close()
            return done()

        dd = Buf(None, "dramdummy")
        k.skip.append(dd)
        for tab_in, c0 in ((p_u, 0), (p_v, D)):
            for i in range(16):
                k.dma("pool", lambda e: e.dma_start(out=UV[i * 1024:(i + 1) * 1024, c0:c0 + D], in_=tab_in[l, i * 1024:(i + 1) * 1024, :]), writes=[dd])
        with ExitStack() as es:
            kt = k.sb(es, "kt", [128, 8, ntok], BF16)
            va = k.sb(es, "vall", [128, NT, VW], BF16)
            for i, qi_ in enumerate((QK_AK, QK_AK + 1, QK_BK, QK_BK + 1, QK_CK, QK_CK + 1, QK_CK + 2, QK_CK + 3)):
                k.dma("sp", lambda e: e.dma_start(out=kt[:, i, :], in_=QK[qi_]), writes=[kt])
            k.dma("sp", lambda e: e.dma_start(out=va[:], in_=VAUG.rearrange("t p w -> p t w")), writes=[va])
            small = k.sb(es, "attsmall", [128, 16], F32)
            lprod = k.sb(es, "lprod", [128, 64], F32)
            subs = k.sb(es, "subs", [128, 128], F32)
            k.op("act", lambda e: e.activation(small[:, 0:8], fpb[:, 0:8], AF.Exp), reads=[fpb], writes=[small])
            for j, (a0, b0) in enumerate(((8, 72), (136, 200))):
                k.op("dve", lambda e: e.tensor_tensor(lprod[:], fpb[:, a0:a0 + 64], fpb[:, b0:b0 + 64], ALU.mult), reads=[fpb], writes=[lprod])
                k.op("dve", lambda e: e.tensor_reduce(small[:, 9 + j:10 + j], lprod[:], AX.X, ALU.add), reads=[lprod], writes=[small])
            k.op("act", lambda e: e.activation(small[:, 9:11], small[:, 9:11], AF.Exp), reads=[small], writes=[small])
            k.op("dve", lambda e: e.tensor_tensor(small[:, 8:9], small[:, 10:11], small[:, 9:10], ALU.subtract), reads=[small], writes=[small])
            k.op("dve", lambda e: e.tensor_scalar(small[:, 8:9], small[:, 8:9], -lam_init, None, ALU.add), reads=[small], writes=[small])
            k.op("dve", lambda e: e.tensor_scalar(subs[:], fpb[:, 264:392], 1.0 - lam_init, None, ALU.mult), reads=[fpb], writes=[subs])

            qgs = Rot([k.sb(es, "qg%d" % i, [128, 4, 512], BF16) for i in range(2)])
            pTs = Rot([k.sb(es, "pT%d" % i, [128, 512], BF16) for i in range(4)])
            otoks = Rot([k.sb(es, "otok%d" % i, [128, 4, 512], F32) for i in range(2)])
            oTs = Rot([k.sb(es, "oT%d" % i, [128, 512], BF16) for i in range(2)])
            rrs = Rot([k.sb(es, "rr%d" % i, [128, 2, 2], F32) for i in range(4)])
            dts_ = Rot([k.sb(es, "dtmp%d" % i, [128, 128], F32) for i in range(2)])
            junk = k.sb(es, "junk", [128, 128], F32)
            sss = Rot([k.sb(es, "ss%d" % i, [128, 1], F32) for i in range(4)])
            Srot = Rot(banks[0:4])
            Orot = Rot(bank2[2:4])

            for mixer in range(3):
                if mixer == 0:
                    qgroups = [[t] for t in range(NT)]
                else:
                    qgroups = [[0, 1]] + [[2 + 4 * i + j for j in range(4)] for i in range(nxt // 4)]
                qbase = (QK_AQ, QK_BQ, QK_CQ)[mixer]
                for qt in qgroups:
                    nq = len(qt)
                    n = nq * 128
                    q0 = qt[0] * 128
                    is_ctx = qt[0] < 2
                    qg = qgs.get()
                    for c in range(4):
                        k.dma("sp" if c % 2 else "act", lambda e: e.dma_start(out=qg[:, c, 0:n], in_=QK[qbase + c, :, q0:q0 + n]), writes=[qg])
                    if is_ctx:
                        keys = [(0, None), (1, None)]
                    elif mixer == 0:
                        t = qt[0]
                        keys = [(0, None), (1, None)]
                        if t - 1 >= 2:
                            keys.append((t - 1, CM_LO))
                        keys.append((t, None))
                        if t + 1 < NT:
                            keys.append((t + 1, CM_U))
                    else:
                        keys = [(j, None) for j in range(NT)]
                    otok = otoks.get()
                    Oprev = None
                    O = None
                    nk = len(keys)
                    steps = [(hd, ji) for hd in range(8) for ji in range(nk)]

                    def headcfg(hd):
                        if mixer == 0:
                            return hd // 4, (hd // 4) * 65, 65
                        elif mixer == 1:
                            return 2 + hd // 4, 130 + (hd // 4) * 65, 65
                        return 4 + hd // 2, 260 + (hd // 2) * 129, 129

                    def emitS(idx):
                        hd, ji = steps[idx]
                        kc = headcfg(hd)[0]
                        kti = keys[ji][0]
                        ps_ = slice((hd % 2) * 64, (hd % 2) * 64 + 64)
                        qc = hd // 2
                        sbk = Srot.get()
                        k.op("pe", lambda e: e.matmul(sbk[:, 0:n], kt[ps_, kc, kti * 128:(kti + 1) * 128], qg[ps_, qc, 0:n], start=True, stop=True),
                             reads=[kt, qg], writes=[sbk])
                        return sbk
                    LA = 2
                    Sq = [emitS(i) for i in range(min(LA, len(steps)))]
                    for idx, (hd, ji) in enumerate(steps):
                        half = hd % 2
                        kc, voff, vw = headcfg(hd)
                        kti, msk = keys[ji]
                        sbk = Sq.pop(0)
                        if idx + LA < len(steps):
                            Sq.append(emitS(idx + LA))
                        if ji == 0:
                            Oprev = O
                            O = Orot.get()
                        pT = pTs.get()
                        k.op("act", lambda e: e.activation(pT[:, 0:n], sbk[:, 0:n], AF.Exp, scale=0.125), reads=[sbk], writes=[pT])
                        if msk is not None:
                            k.op("dve", lambda e: e.tensor_tensor(pT[:, 0:n], pT[:, 0:n], C(msk), ALU.mult), reads=[pT, cm], writes=[pT])

                        def pv(e):
                            for qi in range(nq):
                                ins = e.matmul(O[:, qi // 2, (qi % 2) * 129:(qi % 2) * 129 + vw], pT[:, qi * 128:(qi + 1) * 128],
                                               va[:, kti, voff:voff + vw], start=(ji == 0 and qi % 2 == 0), stop=(ji == nk - 1),
                                               skip_group_check=True)
                            return ins
                        k.op("pe", pv, reads=[pT, va], writes=[O])
                        if ji != nk - 1:
                            continue
                        nb = (nq + 1) // 2
                        npos = min(nq, 2)
                        dcol = vw - 1
                        if mixer < 2:
                            rr = rrs.get()
                            den = O[:, 0:nb, dcol:dcol + 129 * (npos - 1) + 1:129]
                            if mixer == 0:
                                k.op("dve", lambda e: e.tensor_scalar(rr[:, 0:nb, 0:npos], den, small[:, hd:hd + 1], None, ALU.add), reads=[O, small], writes=[rr])
                                k.op("dve", lambda e: e.reciprocal(rr[:, 0:nb, 0:npos], rr[:, 0:nb, 0:npos]), reads=[rr], writes=[rr])
                            else:
                                k.op("dve", lambda e: e.reciprocal(rr[:, 0:nb, 0:npos], den), reads=[O], writes=[rr])
                            for qi in range(nq):
                                k.op("dve", lambda e: e.tensor_scalar(otok[:, qi, hd * 64:(hd + 1) * 64], O[:, qi // 2, (qi % 2) * 129:(qi % 2) * 129 + 64],
                                                                      rr[:, qi // 2, qi % 2:qi % 2 + 1], None, ALU.mult), reads=[O, rr], writes=[otok])
                        elif half == 1:
                            O0, O1 = Oprev, O
                            r0 = rrs.get()
                            r1 = rrs.get()
                            k.op("dve", lambda e: e.reciprocal(r0[:, 0:nb, 0:npos], O0[:, 0:nb, dcol:dcol + 129 * (npos - 1) + 1:129]), reads=[O0], writes=[r0])
                            k.op("dve", lambda e: e.reciprocal(r1[:, 0:nb, 0:npos], O1[:, 0:nb, dcol:dcol + 129 * (npos - 1) + 1:129]), reads=[O1], writes=[r1])
                            k.op("dve", lambda e: e.tensor_scalar(r1[:, 0:nb, 0:npos], r1[:, 0:nb, 0:npos], small[:, 8:9], None, ALU.mult), reads=[r1, small], writes=[r1])
                            hh = hd // 2
                            for qi in range(nq):
                                dtm = dts_.get()
                                ss = sss.get()
                                osl = slice((qi % 2) * 129, (qi % 2) * 129 + 128)
                                k.op("dve", lambda e: e.tensor_scalar(dtm[:], O0[:, qi // 2, osl], r0[:, qi // 2, qi % 2:qi % 2 + 1], None, ALU.mult),
                                     reads=[O0, r0], writes=[dtm])
                                k.op("dve", lambda e: e.scalar_tensor_tensor(dtm[:], O1[:, qi // 2, osl], r1[:, qi // 2, qi % 2:qi % 2 + 1], dtm[:], ALU.mult, ALU.add),
                                     reads=[O1, r1, dtm], writes=[dtm])
                                k.op("pool", lambda e: e.tensor_tensor(junk[:], dtm[:], dtm[:], ALU.mult), reads=[dtm], writes=[junk])
                                k.op("dve", lambda e: e.tensor_reduce(ss[:], junk[:], AX.X, ALU.add), reads=[junk], writes=[ss])
                                k.op("dve", lambda e: e.tensor_scalar(ss[:], ss[:], 1.0 / 128, EPS, ALU.mult, ALU.add), reads=[ss], writes=[ss])
                                k.op("act", lambda e: e.activation(ss[:], ss[:], AF.Ln), reads=[ss], writes=[ss])
                                k.op("act", lambda e: e.activation(ss[:], ss[:], AF.Exp, scale=-0.5), reads=[ss], writes=[ss])
                                k.op("dve", lambda e: e.scalar_tensor_tensor(otok[:, qi, hh * 128:(hh + 1) * 128], dtm[:], ss[:, 0:1], subs[:], ALU.mult, ALU.mult),
                                     reads=[dtm, ss, subs], writes=[otok])
                    for c in range(4):
                        b = Srot.get()

                        def tr(e):
                            for qi in range(nq):
                                ins = e.transpose(b[:, qi * 128:(qi + 1) * 128], otok[:, qi, c * 128:(c + 1) * 128], ident)
                            return ins
                        k.op("pe", tr, reads=[otok, cm], writes=[b])
                        oT = oTs.get()
                        k.op("act", lambda e: e.copy(oT[:, 0:n], b[:, 0:n]), reads=[b], writes=[oT])
                        k.dma("sp", lambda e: e.dma_start(out=OT[mixer * 4 + c, :, q0:q0 + n], in_=oT[:, 0:n]), reads=[oT])
            k.barrier()
        if stop == "B":
            lay.close()
            return done()

        with ExitStack() as es:
            aneg = k.sb(es, "aneg", [128, 32], F32)
            k.op("act", lambda e: e.activation(aneg[:], fpb[:, 424:456], AF.Exp), reads=[fpb], writes=[aneg])
            k.op("dve", lambda e: e.tensor_scalar(aneg[:], aneg[:], -1.0, None, ALU.mult), reads=[aneg], writes=[aneg])
            Hf = k.sb(es, "Hf", [128, 2, 512], F32)
            Hb = k.sb(es, "Hb", [128, 2, 512], BF16)
            xts = Rot([k.sb(es, "xt%d" % i, [128, 1024], BF16) for i in range(3)])
            dtts = Rot([k.sb(es, "dtt%d" % i, [128, 32], F32) for i in range(3)])
            btoks = Rot([k.sb(es, "btk%d" % i, [128, 256], BF16) for i in range(3)])
            bTs = Rot([k.sb(es, "bT%d" % i, [128, 2, 128], BF16) for i in range(3)])
            cTs = Rot([k.sb(es, "cT%d" % i, [128, 2, 128], BF16) for i in range(3)])
            zss = Rot([k.sb(es, "zz%d" % i, [128, 1024], BF16) for i in range(2)])
            yfs = Rot([k.sb(es, "yf%d" % i, [128, 1024], F32) for i in range(2)])
            a_s = Rot([k.sb(es, "a%d" % i, [128, 16], F32) for i in range(2)])
            Es = Rot([k.sb(es, "E%d" % i, [128, 48], F32) for i in range(3)])
            Xds = Rot([k.sb(es, "Xd%d" % i, [128, 1024], BF16) for i in range(3)])
            Xss = Rot([k.sb(es, "Xs%d" % i, [128, 1024], BF16) for i in range(2)])
            cbms = Rot([k.sb(es, "cbm%d" % i, [128, 2, 128], F32) for i in range(2)])
            lts = Rot([k.sb(es, "lt%d" % i, [128, 16, 128], F32) for i in range(2)])
            Lms = Rot([k.sb(es, "Lm%d" % i, [128, 8, 128], F32) for i in range(2)])
            Mts = Rot([k.sb(es, "Mt%d" % i, [128, 8, 128], BF16) for i in range(4)])
            yos = Rot([k.sb(es, "yo%d" % i, [128, 1024], F32) for i in range(2)])
            ytots = Rot([k.sb(es, "ytot%d" % i, [128, 1024], F32) for i in range(2)])
            tmps = Rot([k.sb(es, "ytmp%d" % i, [128, 1024], F32) for i in range(2)])
            rst = Rot([k.sb(es, "rst%d" % i, [128, 2], F32) for i in range(2)])
            sTs = Rot([k.sb(es, "sT%d" % i, [128, 8, 128], BF16) for i in range(2)])
            R1 = Rot(banks[0:2])
            R2 = Rot(bank2[1:4])

            def v3(ap, a, b):
                return ap.rearrange("p (a b) -> p a b", a=a, b=b)

            def ssd_stage1(t, d):
                xt = xts.get(); dtt = dtts.get(); btk = btoks.get(); bT = bTs.get(); cT = cTs.get()
                k.dma("sp", lambda e: e.dma_start(out=xt[:], in_=XTOK[t]), writes=[xt])
                k.dma("sp", lambda e: e.dma_start(out=dtt[:], in_=DT[t]), writes=[dtt])
                k.dma("act", lambda e: e.dma_start(out=btk[:], in_=BTOK[t]), writes=[btk])
                k.dma("act", lambda e: e.dma_start(out=bT[:], in_=BT[:, :, t * 128:(t + 1) * 128].rearrange("g p t -> p g t")), writes=[bT])
                k.dma("act", lambda e: e.dma_start(out=cT[:], in_=CT[:, :, t * 128:(t + 1) * 128].rearrange("g p t -> p g t")), writes=[cT])
                dsl = slice(d * 16, d * 16 + 16)
                tri_i, tri_x, maskl, m01 = (CM_U, CM_LST, CM_LST, CM_U) if d == 0 else (CM_LO, CM_UST, CM_UST, CM_LO)
                a = a_s.get()
                k.op("dve", lambda e: e.tensor_tensor(a[:], dtt[:, dsl], aneg[:, dsl], ALU.mult), reads=[dtt, aneg], writes=[a])
                Xd = Xds.get()
                k.op("dve", lambda e: e.tensor_tensor(v3(Xd[:], 16, 64), v3(xt[:], 16, 64), dtt[:, dsl].unsqueeze(2).to_broadcast([128, 16, 64]), ALU.mult),
                     reads=[xt, dtt], writes=[Xd])
                pc_ = R1.get()

                def cums(e):
                    e.matmul(pc_[:, 0:16], C(tri_i), a[:], start=True, stop=True, skip_group_check=True)
                    e.matmul(pc_[:, 16:32], C(tri_x), a[:], start=False, stop=True, skip_group_check=True)
                    return e.matmul(pc_[:, 32:48], C(CM_ONES), a[:], start=False, stop=True, skip_group_check=True)
                k.op("pe", cums, reads=[a, cm], writes=[pc_])
                E = Es.get()
                k.op("act", lambda e: e.activation(E[:], pc_[:, 0:48], AF.Exp), reads=[pc_], writes=[E])
                pcb = R1.get()

                def cbmm(e):
                    e.matmul(pcb[:, 0:128], bT[:, 0, :], cT[:, 0, :], start=True, stop=True, skip_group_check=True)
                    return e.matmul(pcb[:, 128:256], bT[:, 1, :], cT[:, 1, :], start=False, stop=True, skip_group_check=True)
                k.op("pe", cbmm, reads=[bT, cT], writes=[pcb])
                cbm = cbms.get()
                k.op("dve", lambda e: e.tensor_tensor(cbm[:], v3(pcb[:, 0:256], 2, 128), C(m01).unsqueeze(1).to_broadcast([128, 2, 128]), ALU.mult),
                     reads=[pcb, cm], writes=[cbm])
                lt = lts.get()
                k.op("pool", lambda e: e.tensor_tensor(lt[:], C(maskl).unsqueeze(1).to_broadcast([128, 16, 128]),
                                                       a[:].unsqueeze(2).to_broadcast([128, 16, 128]), ALU.mult), reads=[a, cm], writes=[lt])
                Mtl = []
                for g in range(2):
                    pd = bank2[2]

                    def dmm(e):
                        for ee in range(8):
                            ins = e.matmul(pd[:, ee // 4, (ee % 4) * 128:(ee % 4 + 1) * 128], lt[:, g * 8 + ee, :], C(tri_i),
                                           start=(ee % 4 == 0), stop=True, skip_group_check=True)
                        return ins
                    k.op("pe", dmm, reads=[lt, cm], writes=[pd])
                    Lm = Lms.get()
                    for hb in range(2):
                        k.op("act", lambda e: e.activation(Lm[:, hb * 4:hb * 4 + 4, :], v3(pd[:, hb, :], 4, 128), AF.Exp), reads=[pd], writes=[Lm])
                    Mt = Mts.get()
                    k.op("dve", lambda e: e.tensor_tensor(Mt[:], Lm[:], cbm[:, g, :].unsqueeze(1).to_broadcast([128, 8, 128]), ALU.mult),
                         reads=[Lm, cbm], writes=[Mt])
                    Mtl.append(Mt)
                return dict(xt=xt, btk=btk, cT=cT, E=E, Xd=Xd, Mtl=Mtl)

            def ssd_stage2(t, d, finish, st):
                xt, btk, cT, E, Xd, Mtl = st["xt"], st["btk"], st["cT"], st["E"], st["Xd"], st["Mtl"]
                Y = bank2[1]
                for g in range(2):
                    Mt = Mtl[g]

                    def ymm(e):
                        for ee in range(8):
                            hh = g * 8 + ee
                            ins = e.matmul(Y[:, g, ee * 64:(ee + 1) * 64], Mt[:, ee, :], Xd[:, hh * 64:(hh + 1) * 64],
                                           start=(ee == 0), stop=True, skip_group_check=True)
                        return ins
                    k.op("pe", ymm, reads=[Mt, Xd], writes=[Y])
                Yo = bank2[3]

                def yomm(e):
                    e.matmul(Yo[:, 0, :], cT[:, 0, :], Hb[:, 0, :], start=True, stop=True, skip_group_check=True)
                    return e.matmul(Yo[:, 1, :], cT[:, 1, :], Hb[:, 1, :], start=True, stop=True, skip_group_check=True)
                k.op("pe", yomm, reads=[cT, Hb], writes=[Yo])
                yo = yos.get()
                k.op("dve", lambda e: e.tensor_tensor(v3(yo[:], 16, 64), Yo[:].rearrange("p g (e q) -> p (g e) q", q=64),
                                                      E[:, 0:16].unsqueeze(2).to_broadcast([128, 16, 64]), ALU.mult), reads=[Yo, E], writes=[yo])
                Xs = Xss.get()
                k.op("pool", lambda e: e.tensor_tensor(v3(Xs[:], 16, 64), v3(Xd[:], 16, 64), E[:, 16:32].unsqueeze(2).to_broadcast([128, 16, 64]), ALU.mult),
                     reads=[Xd, E], writes=[Xs])
                Hn = bank2[3]

                def hmm(e):
                    e.matmul(Hn[:, 0, :], btk[:, 0:128], Xs[:, 0:512], start=True, stop=True, skip_group_check=True)
                    return e.matmul(Hn[:, 1, :], btk[:, 128:256], Xs[:, 512:1024], start=True, stop=True, skip_group_check=True)
                k.op("pe", hmm, reads=[btk, Xs], writes=[Hn])
                k.op("dve", lambda e: e.tensor_tensor(Hf[:].rearrange("p g (e q) -> p (g e) q", q=64), Hf[:].rearrange("p g (e q) -> p (g e) q", q=64),
                                                      E[:, 32:48].unsqueeze(2).to_broadcast([128, 16, 64]), ALU.mult), reads=[Hf, E], writes=[Hf])
                k.op("dve", lambda e: e.tensor_tensor(Hf[:], Hf[:], Hn[:], ALU.add), reads=[Hf, Hn], writes=[Hf])
                k.op("pool", lambda e: e.tensor_copy(Hb[:], Hf[:]), reads=[Hf], writes=[Hb])
                if not finish:
                    yf = yfs.get()
                    k.op("dve", lambda e: e.tensor_tensor(yf[:], Y[:].rearrange("p g c -> p (g c)"), yo[:], ALU.add), reads=[Y, yo], writes=[yf])
                    k.dma("sp", lambda e: e.dma_start(out=YF[t], in_=yf[:]), reads=[yf])
                    return
                yf = yfs.get(); zz = zss.get()
                k.dma("sp", lambda e: e.dma_start(out=yf[:], in_=YF[t]), writes=[yf])
                k.dma("sp", lambda e: e.dma_start(out=zz[:], in_=ZS[t]), writes=[zz])
                yt = ytots.get(); tm = tmps.get()
                k.op("dve", lambda e: e.tensor_tensor(yt[:], Y[:].rearrange("p g c -> p (g c)"), yo[:], ALU.add), reads=[Y, yo], writes=[yt])
                k.op("pool", lambda e: e.tensor_tensor(yt[:], yt[:], yf[:], ALU.add), reads=[yt, yf], writes=[yt])
                k.op("pool", lambda e: e.tensor_tensor(v3(tm[:], 16, 64), v3(xt[:], 16, 64), fpb[:, 456:472].unsqueeze(2).to_broadcast([128, 16, 64]), ALU.mult),
                     reads=[xt, fpb], writes=[tm])
                k.op("pool", lambda e: e.tensor_tensor(yt[:], yt[:], tm[:], ALU.add), reads=[yt, tm], writes=[yt])
                k.op("dve", lambda e: e.tensor_tensor(yt[:], yt[:], zz[:], ALU.mult), reads=[yt, zz], writes=[yt])
                k.op("pool", lambda e: e.tensor_tensor(tm[:], yt[:], yt[:], ALU.mult), reads=[yt], writes=[tm])
                rs_ = rst.get()
                k.op("dve", lambda e: e.tensor_reduce(rs_[:], v3(tm[:], 2, 512), AX.X, ALU.add), reads=[tm], writes=[rs_])
                k.op("dve", lambda e: e.tensor_scalar(rs_[:], rs_[:], 1.0 / 512, EPS, ALU.mult, ALU.add), reads=[rs_], writes=[rs_])
                k.op("act", lambda e: e.activation(rs_[:], rs_[:], AF.Sqrt), reads=[rs_], writes=[rs_])
                k.op("dve", lambda e: e.reciprocal(rs_[:], rs_[:]), reads=[rs_], writes=[rs_])
                k.op("dve", lambda e: e.tensor_tensor(v3(yt[:], 2, 512), v3(yt[:], 2, 512), rs_[:].unsqueeze(2).to_broadcast([128, 2, 512]), ALU.mult),
                     reads=[yt, rs_], writes=[yt])
                k.op("pool", lambda e: e.tensor_tensor(yt[:], yt[:], fpb[:, 472:1496], ALU.mult), reads=[yt, fpb], writes=[yt])
                sT = sTs.get()
                for hb in range(2):
                    b = R1.get()

                    def tr(e):
                        for q in range(4):
                            cc = hb * 4 + q
                            ins = e.transpose(b[:, q * 128:(q + 1) * 128], yt[:, cc * 128:(cc + 1) * 128], ident)
                        return ins
                    k.op("pe", tr, reads=[yt, cm], writes=[b])
                    k.op("act", lambda e: e.copy(sT[:, hb * 4:hb * 4 + 4, :], v3(b[:], 4, 128)), reads=[b], writes=[sT])
                k.dma("sp", lambda e: e.dma_start(out=OT[12:20, :, t * 128:(t + 1) * 128].rearrange("c p t -> p c t"), in_=sT[:]), reads=[sT])

            for d in range(2):
                k.op("pool", lambda e: e.memset(Hf[:], 0.0), writes=[Hf])
                k.op("pool", lambda e: e.memset(Hb[:], 0.0), writes=[Hb])
                order = list(range(NT)) if d == 0 else [1, 0] + list(range(NT - 1, 1, -1))
                st = ssd_stage1(order[0], d)
                for i, t in enumerate(order):
                    nst = ssd_stage1(order[i + 1], d) if i + 1 < len(order) else None
                    ssd_stage2(t, d, d == 1, st)
                    st = nst
                k.barrier()
        if stop == "D":
            lay.close()
            return done()

        with ExitStack() as es:
            wbr = k.sb(es, "wbr", [128, 20, 1024], BF16)
            wo = k.sb(es, "wo", [128, 8, 1024], BF16)
            for i in range(4):
                k.dma("pool", lambda e: e.dma_start(out=wbr[:, i * 5:(i + 1) * 5, :], in_=w_br[l].rearrange("(f p) d -> p f d", p=128)[:, i * 5:(i + 1) * 5, :]), writes=[wbr])
            k.dma("pool", lambda e: e.dma_start(out=wo[:], in_=w_out[l].rearrange("(f p) d -> p f d", p=128)), writes=[wo])
            oTg = k.sb(es, "oTg", [128, 20, 512], BF16)
            gTg = k.sb(es, "gTg", [128, 32, 512], BF16)
            xTg = k.sb(es, "xTg", [128, 8, 512], F32)
            accf = k.sb(es, "accf", [128, 8, 512], F32)
            accb = k.sb(es, "accb", [128, 8, 512], BF16)
            tmpE = Rot([k.sb(es, "tmpE%d" % i, [128, 512], F32) for i in range(2)])
            sqE = accf
            rsE = k.sb(es, "rsE", [128, 512], F32)
            tTg = k.sb(es, "tTg", [128, 8, 512], BF16)
            bk = Rot(banks)
            brc = ((0, 4), (4, 8), (8, 12), (12, 20))
            for (t0, n) in groups:
                w = 1 if t0 == 0 else 0
                k.dma("sp", lambda e: e.dma_start(out=oTg[:, :, 0:n], in_=OT[:, :, t0:t0 + n].rearrange("c p t -> p c t")), writes=[oTg])
                k.dma("act", lambda e: e.dma_start(out=gTg[:, :, 0:n], in_=GT[:, :, t0:t0 + n].rearrange("c p t -> p c t")), writes=[gTg])
                k.dma("sp", lambda e: e.dma_start(out=xTg[:, :, 0:n], in_=XT[:, :, t0:t0 + n].rearrange("c p t -> p c t")), writes=[xTg])
                for dm in range(8):
                    for br in range(4):
                        b = bk.get()
                        f0, f1 = brc[br]

                        def mm(e):
                            for fc in range(f0, f1):
                                ins = e.matmul(b[:, 0:n], wbr[:, fc, dm * 128:(dm + 1) * 128], oTg[:, fc, 0:n], start=(fc == f0), stop=(fc == f1 - 1))
                            return ins
                        k.op("pe", mm, reads=[wbr, oTg], writes=[b])
                        if br == 0:
                            k.op("dve", lambda e: e.tensor_tensor(accf[:, dm, 0:n], b[:, 0:n], gTg[:, br * 8 + dm, 0:n], ALU.mult), reads=[b, gTg], writes=[accf])
                        else:
                            tb = tmpE.get()
                            k.op("dve", lambda e: e.tensor_tensor(tb[:, 0:n], b[:, 0:n], gTg[:, br * 8 + dm, 0:n], ALU.mult), reads=[b, gTg], writes=[tb])
                            if br < 3:
                                k.op("pool", lambda e: e.tensor_tensor(accf[:, dm, 0:n], accf[:, dm, 0:n], tb[:, 0:n], ALU.add), reads=[accf, tb], writes=[accf])
                            else:
                                k.op("pool", lambda e: e.tensor_tensor(accb[:, dm, 0:n], accf[:, dm, 0:n], tb[:, 0:n], ALU.add), reads=[accf, tb], writes=[accb])
                for dm2 in range(8):
                    b = bk.get()

                    def mm(e):
                        for dm in range(8):
                            ins = e.matmul(b[:, 0:n], wo[:, dm, dm2 * 128:(dm2 + 1) * 128], accb[:, dm, 0:n], start=(dm == 0), stop=(dm == 7))
                        return ins
                    k.op("pe", mm, reads=[wo, accb], writes=[b])
                    k.op("dve", lambda e: e.scalar_tensor_tensor(xTg[:, dm2, 0:n], b[:, 0:n], modcol(2, dm2, w), xTg[:, dm2, 0:n], ALU.mult, ALU.add),
                         reads=[b, mods, xTg], writes=[xTg])
                k.dma("sp", lambda e: e.dma_start(out=XT[:, :, t0:t0 + n].rearrange("c p t -> p c t"), in_=xTg[:, :, 0:n]), reads=[xTg])
                k.op("act", lambda e: e.activation(sqE[:, :, 0:n], xTg[:, :, 0:n], AF.Square), reads=[xTg], writes=[sqE])
                b = bk.get()

                def mm(e):
                    for c in range(8):
                        ins = e.matmul(b[:, 0:n], C(CM_ONES), sqE[:, c, 0:n], start=(c == 0), stop=(c == 7))
                    return ins
                k.op("pe", mm, reads=[sqE, cm], writes=[b])
                k.op("dve", lambda e: e.tensor_scalar(rsE[:, 0:n], b[:, 0:n], 1.0 / D, EPS, ALU.mult, ALU.add), reads=[b], writes=[rsE])
                k.op("act", lambda e: e.activation(rsE[:, 0:n], rsE[:, 0:n], AF.Sqrt), reads=[rsE], writes=[rsE])
                k.op("dve", lambda e: e.reciprocal(rsE[:, 0:n], rsE[:, 0:n]), reads=[rsE], writes=[rsE])
                for c in range(8):
                    tb = tmpE.get()
                    k.op("dve", lambda e: e.scalar_tensor_tensor(tb[:, 0:n], xTg[:, c, 0:n], gs2[:, c, w:w + 1], rsE[:, 0:n], ALU.mult, ALU.mult),
                         reads=[xTg, gs2, rsE], writes=[tb])
                    k.op("act", lambda e: e.activation(tTg[:, c, 0:n], tb[:, 0:n], AF.Identity, bias=modcol(3, c, w), scale=1.0),
                         reads=[tb, mods], writes=[tTg])
                k.dma("sp", lambda e: e.dma_start(out=TT[:, :, t0:t0 + n].rearrange("c p t -> p c t"), in_=tTg[:, :, 0:n]), reads=[tTg])
            k.barrier()
        if stop == "E":
            lay.close()
            return done()

        with ExitStack() as es:
            wq = k.sb(es, "wq", [128, 8, 2048], BF16)
            sk = k.sb(es, "sk", [128, 16, 128], BF16)
            for i in range(2):
                k.dma("pool", lambda e: e.dma_start(out=wq[:, i * 4:(i + 1) * 4, :], in_=p_wq[l].rearrange("(f p) d -> p f d", p=128)[:, i * 4:(i + 1) * 4, :]), writes=[wq])
            k.dma("pool", lambda e: e.dma_start(out=sk[:], in_=skt_in[l]), writes=[sk])
            identb = k.sb(es, "identb", [128, 128], BF16)
            iota16 = k.sb(es, "iota16", [128, 16], F32)
            k.op("dve", lambda e: e.tensor_copy(identb[:], ident), reads=[cm], writes=[identb])
            for i in range(16):
                k.op("dve", lambda e: e.memset(iota16[:, i:i + 1], float(i)), writes=[iota16])
            tTs = Rot([k.sb(es, "tT%d" % i, [128, 8, 128], BF16) for i in range(2)])
            tTf = k.sb(es, "tTf", [128, 8, 128], F32)
            ttoks = Rot([k.sb(es, "ttok%d" % i, [128, 1024], BF16) for i in range(2)])
            qTs = k.sb(es, "qTs", [128, 16, 128], BF16)
            S1 = k.sb(es, "S1", [128, 16, 128], F32)
            S2 = k.sb(es, "S2", [128, 16, 128], F32)
            sv = k.sb(es, "sv", [128, 16, 16], F32)
            si = k.sb(es, "si", [128, 16, 16], U32)
            sif = k.sb(es, "sif", [128, 16, 16], F32)
            cand = k.sb(es, "cand", [128, 8, 256], F32)
            cand2 = k.sb(es, "cand2", [128, 8, 256], F32)
            bv = k.sb(es, "bv", [128, 8, 16], F32)
            pos = k.sb(es, "pos", [128, 8, 16], U32)
            pa = k.sb(es, "pa", [128, 8, 16], U32)
            pb = k.sb(es, "pb", [128, 8, 16], U32)
            paf = k.sb(es, "paf", [128, 8, 16], F32)
            pbf = k.sb(es, "pbf", [128, 8, 16], F32)
            oh = k.sb(es, "oh", [128, 8, 256], F32)
            sel0 = k.sb(es, "sel0", [128, 8, 16], F32)
            sel1 = k.sb(es, "sel1", [128, 8, 16], F32)
            eidx = Rot([k.sb(es, "eidx%d" % i, [128, 128], I32) for i in range(2)])
            gates_ = Rot([k.sb(es, "gate%d" % i, [128, 8, 16], F32) for i in range(2)])
            gsum = k.sb(es, "gsum", [128, 8], F32)
            NG = 16
            guv = Rot([k.sb(es, "guv%d" % i, [128, 2048], BF16) for i in range(NG)])
            actgs = Rot([k.sb(es, "actg%d" % i, [128, 8], F32) for i in range(4)])
            for ab in actgs.bufs:
                ab.cols = [Buf(ab.t[:, j:j + 1], "agc") for j in range(8)]
            wgs8 = Rot([k.sb(es, "wg8_%d" % i, [128, 8], F32) for i in range(4)])
            junks = Rot([k.sb(es, "junkF%d" % i, [128, 1024], BF16) for i in range(6)])
            dgs = Rot([k.sb(es, "dg%d" % i, [128, 128], BF16) for i in range(8)])
            accs = k.sb(es, "accs", [128, 1024], F32)
            xTt = Rot([k.sb(es, "xTt%d" % i, [128, 8, 128], F32) for i in range(2)])
            R1 = Rot(banks[0:4])
            ACC = bank2[2]
            k.skip.remove(dd)
            k.barrier()
            k.release([dd])

            def v3(ap, a, b):
                return ap.rearrange("p (a b) -> p a b", a=a, b=b)

            def stageA(t, st):
                tT = tTs.get()
                k.dma("sp", lambda e: e.dma_start(out=tT[:], in_=TT[:, :, t * 128:(t + 1) * 128].rearrange("c p t -> p c t")), writes=[tT])
                k.op("act", lambda e: e.copy(tTf[:], tT[:]), reads=[tT], writes=[tTf])
                ttok = ttoks.get()
                for hb in range(2):
                    b = R1.get()

                    def tr(e):
                        for q in range(4):
                            ins = e.transpose(b[:, q * 128:(q + 1) * 128], tTf[:, hb * 4 + q, :], ident)
                        return ins
                    k.op("pe", tr, reads=[tTf, cm], writes=[b])
                    k.op("act", lambda e: e.copy(ttok[:, hb * 512:(hb + 1) * 512], b[:]), reads=[b], writes=[ttok])
                yield
                for q4 in range(4):
                    b = R1.get()

                    def mm(e):
                        for j in range(4):
                            hj = q4 * 4 + j
                            for kk in range(8):
                                ins = e.matmul(b[:, j * 128:(j + 1) * 128], wq[:, kk, hj * 128:(hj + 1) * 128], tT[:, kk, :],
                                               start=(kk == 0 and j == 0), stop=(kk == 7), skip_group_check=True)
                        return ins
                    k.op("pe", mm, reads=[wq, tT], writes=[b])
                    k.op("act", lambda e: e.copy(qTs[:, q4 * 4:q4 * 4 + 4, :], v3(b[:], 4, 128)), reads=[b], writes=[qTs])
                for q4 in range(4):
                    b = R1.get()

                    def mm(e):
                        for j in range(4):
                            hj = q4 * 4 + j
                            ins = e.matmul(b[:, j * 128:(j + 1) * 128], qTs[:, hj, :], sk[:, hj, :], start=(j == 0), stop=True, skip_group_check=True)
                        return ins
                    k.op("pe", mm, reads=[qTs, sk], writes=[b])
                    k.op("act", lambda e: e.copy(S1[:, q4 * 4:q4 * 4 + 4, :], v3(b[:], 4, 128)), reads=[b], writes=[S1])
                yield
                for hj in range(16):
                    k.op("dve", lambda e: e.max(out=sv[:, hj, 0:8], in_=S1[:, hj, :]), reads=[S1], writes=[sv])
                    k.op("dve", lambda e: e.max_index(out=si[:, hj, 0:8], in_max=sv[:, hj, 0:8], in_values=S1[:, hj, :]), reads=[S1, sv], writes=[si])
                    k.op("dve", lambda e: e.match_replace(out=S2[:, hj, :], in_to_replace=sv[:, hj, 0:8], in_values=S1[:, hj, :], imm_value=-1e30),
                         reads=[S1, sv], writes=[S2])
                    k.op("dve", lambda e: e.max(out=sv[:, hj, 8:16], in_=S2[:, hj, :]), reads=[S2], writes=[sv])
                    k.op("dve", lambda e: e.max_index(out=si[:, hj, 8:16], in_max=sv[:, hj, 8:16], in_values=S2[:, hj, :]), reads=[S2, sv], writes=[si])
                    if hj % 2 == 1:
                        yield
                k.op("dve", lambda e: e.tensor_copy(sif[:], si[:]), reads=[si], writes=[sif])
                sv4 = sv[:].rearrange("p (h j) a -> p h j a", j=2)
                sif4 = sif[:].rearrange("p (h j) a -> p h j a", j=2)
                k.op("dve", lambda e: e.tensor_tensor(cand[:].rearrange("p h (a b) -> p h a b", a=16), sv4[:, :, 0, :].unsqueeze(3).to_broadcast([128, 8, 16, 16]),
                                                      sv4[:, :, 1, :].unsqueeze(2).to_broadcast([128, 8, 16, 16]), ALU.add), reads=[sv], writes=[cand])
                for h in range(8):
                    k.op("dve", lambda e: e.max(out=bv[:, h, 0:8], in_=cand[:, h, :]), reads=[cand], writes=[bv])
                    k.op("dve", lambda e: e.max_index(out=pos[:, h, 0:8], in_max=bv[:, h, 0:8], in_values=cand[:, h, :]), reads=[cand, bv], writes=[pos])
                    k.op("dve", lambda e: e.match_replace(out=cand2[:, h, :], in_to_replace=bv[:, h, 0:8], in_values=cand[:, h, :], imm_value=-1e30),
                         reads=[cand, bv], writes=[cand2])
                    k.op("dve", lambda e: e.max(out=bv[:, h, 8:16], in_=cand2[:, h, :]), reads=[cand2], writes=[bv])
                    k.op("dve", lambda e: e.max_index(out=pos[:, h, 8:16], in_max=bv[:, h, 8:16], in_values=cand2[:, h, :]), reads=[cand2, bv], writes=[pos])
                    if h % 2 == 1:
                        yield
                k.op("dve", lambda e: e.tensor_scalar(pa[:], pos[:], 4, None, ALU.logical_shift_right), reads=[pos], writes=[pa])
                k.op("dve", lambda e: e.tensor_scalar(pb[:], pos[:], 15, None, ALU.bitwise_and), reads=[pos], writes=[pb])
                k.op("dve", lambda e: e.tensor_copy(paf[:], pa[:]), reads=[pa], writes=[paf])
                k.op("dve", lambda e: e.tensor_copy(pbf[:], pb[:]), reads=[pb], writes=[pbf])
                yield
                oh4 = oh[:].rearrange("p h (r a) -> p h r a", r=16)
                io4 = iota16[:].unsqueeze(1).unsqueeze(1).to_broadcast([128, 8, 16, 16])
                for (pf, jj, sel) in ((paf, 0, sel0), (pbf, 1, sel1)):
                    k.op("dve", lambda e: e.tensor_tensor(oh4, pf[:].unsqueeze(3).to_broadcast([128, 8, 16, 16]), io4, ALU.is_equal), reads=[pf, iota16], writes=[oh])
                    k.op("dve", lambda e: e.tensor_tensor(oh4, oh4, sif4[:, :, jj, :].unsqueeze(2).to_broadcast([128, 8, 16, 16]), ALU.mult), reads=[oh, sif], writes=[oh])
                    k.op("dve", lambda e: e.tensor_reduce(sel[:], oh4, AX.X, ALU.add), reads=[oh], writes=[sel])
                    yield
                yield
                ei = eidx.get()
                gate = gates_.get()
                k.op("dve", lambda e: e.scalar_tensor_tensor(sel0[:], sel0[:], 128.0, sel1[:], ALU.mult, ALU.add), reads=[sel0, sel1], writes=[sel0])
                k.op("dve", lambda e: e.tensor_copy(ei[:], sel0[:].rearrange("p h r -> p (h r)")), reads=[sel0], writes=[ei])
                k.op("dve", lambda e: e.tensor_tensor(gate[:], bv[:], bv[:, :, 0:1].to_broadcast([128, 8, 16]), ALU.subtract), reads=[bv], writes=[gate])
                k.op("act", lambda e: e.activation(gate[:], gate[:], AF.Exp), reads=[gate], writes=[gate])
                k.op("dve", lambda e: e.tensor_reduce(gsum[:], gate[:], AX.X, ALU.add), reads=[gate], writes=[gsum])
                k.op("dve", lambda e: e.reciprocal(gsum[:], gsum[:]), reads=[gsum], writes=[gsum])
                k.op("dve", lambda e: e.tensor_tensor(gate[:], gate[:], gsum[:].unsqueeze(2).to_broadcast([128, 8, 16]), ALU.mult), reads=[gate, gsum], writes=[gate])
                st.update(ttok=ttok, ei=ei, gate=gate)

            def stageUV(t, st, gen):
                w = 1 if t < 2 else 0
                ttok, ei, gate = st['ttok'], st['ei'], st['gate']
                gflat = gate[:].rearrange("p h r -> p (h r)")
                xt_ = xTt.get()
                k.dma("sp", lambda e: e.dma_start(out=xt_[:], in_=XT[:, :, t * 128:(t + 1) * 128].rearrange("c p t -> p c t")), writes=[xt_])

                for g in range(16):
                    gb = []
                    ag = actgs.get()
                    for j in range(8):
                        m = g * 8 + j
                        gu = guv.get()
                        jk = junks.get()
                        k.dma("pool", lambda e: e.indirect_dma_start(out=gu[:], out_offset=None, in_=UV,
                                                                     in_offset=bass.IndirectOffsetOnAxis(ap=ei[:, m:m + 1], axis=0)), reads=[ei], writes=[gu])
                        if j in (2, 6):
                            k.op("dve", lambda e: e.tensor_tensor(jk[:], gu[:, 0:1024], ttok[:], ALU.mult), reads=[gu, ttok], writes=[jk])
                            k.op("act", lambda e: e.activation(jk[:], jk[:], AF.Copy, accum_out=ag.t[:, j:j + 1]), reads=[jk], writes=[jk, ag.cols[j]])
                        else:
                            k.op("dve", lambda e: e.scalar_tensor_tensor(jk[:], gu[:, 0:1024], 1.0, ttok[:], ALU.mult, ALU.mult, accum_out=ag.t[:, j:j + 1]),
                                 reads=[gu, ttok], writes=[jk, ag.cols[j]])
                        gb.append(gu)
                    k.op("act", lambda e: e.activation(ag.t[:], ag.t[:], AF.Gelu), reads=ag.cols, writes=ag.cols)
                    w8 = wgs8.get()
                    for j in range(8):
                        m = g * 8 + j
                        k.op("act", lambda e: e.activation(w8[:, j:j + 1], ag.t[:, j:j + 1], AF.Copy, scale=gflat[:, m:m + 1]), reads=[ag.cols[j], gate], writes=[w8])
                    for j in range(8):
                        m = g * 8 + j
                        gv = gb[j]
                        dg = dgs.get()
                        k.op("act", lambda e: e.activation(dg[:], identb[:], AF.Copy, scale=w8[:, j:j + 1]), reads=[identb, w8], writes=[dg])

                        def mm(e):
                            e.matmul(ACC[:, 0, :], dg[:], gv[:, 1024:1536], start=(m == 0), stop=(m == 127), skip_group_check=True)
                            return e.matmul(ACC[:, 1, :], dg[:], gv[:, 1536:2048], start=(m == 0), stop=(m == 127), skip_group_check=True)
                        k.op("pe", mm, reads=[dg, gv], writes=[ACC])
                    if gen is not None and g >= 1:
                        for _ in range(2):
                            next(gen, None)
                k.op("act", lambda e: e.copy(accs[:], ACC[:].rearrange("p g c -> p (g c)")), reads=[ACC], writes=[accs])
                for hb in range(2):
                    b = R1.get()

                    def tr(e):
                        for q in range(4):
                            cc = hb * 4 + q
                            ins = e.transpose(b[:, q * 128:(q + 1) * 128], accs[:, cc * 128:(cc + 1) * 128], ident)
                        return ins
                    k.op("pe", tr, reads=[accs, cm], writes=[b])
                    for q in range(4):
                        cc = hb * 4 + q
                        k.op("dve", lambda e: e.scalar_tensor_tensor(xt_[:, cc, :], b[:, q * 128:(q + 1) * 128], modcol(5, cc, w), xt_[:, cc, :], ALU.mult, ALU.add),
                             reads=[b, mods, xt_], writes=[xt_])
                k.dma("sp", lambda e: e.dma_start(out=XT[:, :, t * 128:(t + 1) * 128].rearrange("c p t -> p c t"), in_=xt_[:]), reads=[xt_])

            cur = {}
            for _ in stageA(0, cur):
                pass
            for t in range(NT):
                nst = {}
                gen = stageA(t + 1, nst) if t + 1 < NT else None
                stageUV(t, cur, gen)
                if gen is not None:
                    for _ in gen:
                        pass
                cur = nst
            k.barrier()
        lay.close()
        if stop == "F" and l == 0:
            return done()

    with ExitStack() as es:
        gf = k.sb(es, "gf", [128, 8], F32)
        k.dma("sp", lambda e: e.dma_start(out=gf[:], in_=gfin_in), writes=[gf])
        xg = Rot([k.sb(es, "gxg%d" % i, [128, 8, 512], F32) for i in range(2)])
        sq = k.sb(es, "gsq", [128, 8, 512], F32)
        rs = k.sb(es, "grs", [128, 512], F32)
        yT = k.sb(es, "gyT", [128, 8, 512], F32)
        ots = Rot([k.sb(es, "got%d" % i, [128, 1024], F32) for i in range(2)])
        bk = Rot(banks)
        for (t0, n) in groups[1:]:
            x = xg.get()
            k.dma("sp", lambda e: e.dma_start(out=x[:, :, 0:n], in_=XT[:, :, t0:t0 + n].rearrange("c p t -> p c t")), writes=[x])
            k.op("act", lambda e: e.activation(sq[:, :, 0:n], x[:, :, 0:n], AF.Square), reads=[x], writes=[sq])
            b = bk.get()

            def mm(e):
                for c in range(8):
                    ins = e.matmul(b[:, 0:n], C(CM_ONES), sq[:, c, 0:n], start=(c == 0), stop=(c == 7))
                return ins
            k.op("pe", mm, reads=[sq, cm], writes=[b])
            k.op("dve", lambda e: e.tensor_scalar(rs[:, 0:n], b[:, 0:n], 1.0 / D, EPS, ALU.mult, ALU.add), reads=[b], writes=[rs])
            k.op("act", lambda e: e.activation(rs[:, 0:n], rs[:, 0:n], AF.Sqrt), reads=[rs], writes=[rs])
            k.op("dve", lambda e: e.reciprocal(rs[:, 0:n], rs[:, 0:n]), reads=[rs], writes=[rs])
            for c in range(8):
                k.op("dve", lambda e: e.scalar_tensor_tensor(yT[:, c, 0:n], x[:, c, 0:n], gf[:, c:c + 1], rs[:, 0:n], ALU.mult, ALU.mult),
                     reads=[x, gf, rs], writes=[yT])
            for q in range(n // 128):
                ot = ots.get()
                for hb in range(2):
                    b = bk.get()

                    def tr(e):
                        for j in range(4):
                            ins = e.transpose(b[:, j * 128:(j + 1) * 128], yT[:, hb * 4 + j, q * 128:(q + 1) * 128], ident)
                        return ins
                    k.op("pe", tr, reads=[yT, cm], writes=[b])
                    k.op("act" if hb else "dve", lambda e: (e.copy if hb else e.tensor_copy)(ot[:, hb * 512:(hb + 1) * 512], b[:]), reads=[b], writes=[ot])
                r0 = t0 - 256 + q * 128
                k.dma("sp", lambda e: e.dma_start(out=out[r0:r0 + 128, :], in_=ot[:]), reads=[ot])
        k.barrier()
    return done()


_CACHE = {}


def kernel(**inputs):
    L, nxt = 4, 32
    B = 8
    inp = {kk: np.asarray(v) for kk, v in inputs.items()}
    shared = host_prep(inp, L, nxt)
    if "nc" not in _CACHE:
        _CACHE["nc"] = build(L, nxt)
    nc = _CACHE["nc"]
    in_maps = []
    for b in range(B):
        m = dict(shared)
        m["x"] = np.ascontiguousarray(inp["x"][b], np.float32)
        m["ctx"] = np.ascontiguousarray(inp["ctx"][b], np.float32)
        cv = np.stack([np.asarray(inp["c"][b], np.float32).reshape(8, 128).T,
                       np.asarray(inp["c_ctx"], np.float32).reshape(8, 128).T], axis=-1)
        m["cvec"] = np.ascontiguousarray(cv)
        in_maps.append(m)
    res = run_bass_kernel_spmd(nc, in_maps, core_ids=list(range(B)))
    return np.stack([np.asarray(r["out"], np.float32) for r in res.results], axis=0)
```
